# Optimizing a Trainium2 kernel written in Bass

```python
import jax, jax.numpy as jnp
from jax import lax
import numpy as np

D_MODEL = 2048
BATCH = 16
SEQ = 2048
DEPTH = 1

D_CONV = D_MODEL // 2
CONV_HEADS = 16
CONV_HEAD_DIM = D_CONV // CONV_HEADS
CONV_WIDTH = 3
D_LRU = D_MODEL // 2
LRU_HEADS = 8
LRU_HEAD_DIM = D_LRU // LRU_HEADS
LRU_CONV_WIDTH = 4
LRU_C = 8.0
D_MIX = D_CONV + D_LRU
D_IN = 3 * D_CONV + 2 * D_LRU
N_GROUPS = 8
EXPERTS_PER_GROUP = 8
N_EXPERTS = N_GROUPS * EXPERTS_PER_GROUP
TOP_K = 2
D_EXPERT = D_MODEL // 4
EXPERT_BLOCK = 128
EPS = 1e-6

kernel_name = "hybrid_conv_rglru_hiermoe_adaln"


def rms_normalize(x):
    xf = x.astype(jnp.float32)
    return xf * lax.rsqrt(jnp.mean(xf * xf, axis=-1, keepdims=True) + EPS)


def rmsnorm(x, g):
    return (rms_normalize(x) * g.astype(jnp.float32)).astype(x.dtype)


def modulated_rmsnorm(x, g, shift, scale):
    y = rms_normalize(x) * g.astype(jnp.float32)
    y = y * (1.0 + scale.astype(jnp.float32)[:, None, :]) + shift.astype(jnp.float32)[:, None, :]
    return y.astype(x.dtype)


def head_rmsnorm(y, g, n_heads, head_dim):
    b, s, _ = y.shape
    yn = rms_normalize(y.reshape(b, s, n_heads, head_dim)).reshape(b, s, n_heads * head_dim)
    return (yn * g.astype(jnp.float32)).astype(y.dtype)


def causal_depthwise_conv(u, w):
    k, ch = w.shape
    return lax.conv_general_dilated(
        u, w[:, None, :].astype(u.dtype), window_strides=(1,), padding=[(k - 1, 0)],
        dimension_numbers=("NWC", "WIO", "NWC"), feature_group_count=ch)


def rg_lru(xb, w_a, b_a, w_x, b_x, lam):
    bsz, s, dm = xb.shape
    xh = xb.reshape(bsz, s, LRU_HEADS, LRU_HEAD_DIM)
    r = jax.nn.sigmoid((jnp.einsum("bshi,hij->bshj", xh, w_a).reshape(bsz, s, dm) + b_a).astype(jnp.float32))
    i = jax.nn.sigmoid((jnp.einsum("bshi,hij->bshj", xh, w_x).reshape(bsz, s, dm) + b_x).astype(jnp.float32))
    log_a = -LRU_C * r * jax.nn.softplus(-lam.astype(jnp.float32))
    a = jnp.exp(log_a)
    mult = jnp.sqrt(-jnp.expm1(2.0 * log_a))
    u = mult * i * xb.astype(jnp.float32)

    def step(h, inp):
        a_t, u_t = inp
        h = a_t * h + u_t
        return h, h

    h0 = jnp.zeros((bsz, dm), jnp.float32)
    _, hs = lax.scan(step, h0, (jnp.swapaxes(a, 0, 1), jnp.swapaxes(u, 0, 1)))
    return jnp.swapaxes(hs, 0, 1).astype(xb.dtype)


def token_mix(h, w_in, conv3_w, conv4_w, conv4_b, lru_w_a, lru_b_a, lru_w_x, lru_b_x, lru_lambda,
              head_norm_conv_g, head_norm_lru_g, w_out):
    proj = h @ w_in
    b_a, c_a, x_a, g_b, x_b = jnp.split(
        proj, [D_CONV, 2 * D_CONV, 3 * D_CONV, 3 * D_CONV + D_LRU], axis=-1)
    y_a = b_a * causal_depthwise_conv(c_a * x_a, conv3_w)
    x_b = causal_depthwise_conv(x_b, conv4_w) + conv4_b.astype(x_b.dtype)
    y_b = rg_lru(x_b, lru_w_a, lru_b_a, lru_w_x, lru_b_x, lru_lambda) * jax.nn.gelu(g_b)
    y = jnp.concatenate([
        head_rmsnorm(y_a, head_norm_conv_g, CONV_HEADS, CONV_HEAD_DIM),
        head_rmsnorm(y_b, head_norm_lru_g, LRU_HEADS, LRU_HEAD_DIM)], axis=-1)
    return y @ w_out


def hier_moe(h, route_w_group, route_b_group, route_w_expert, route_b_expert, w_e_gate, w_e_up, w_e_down):
    n, d = h.shape
    group_logits = (h @ route_w_group).astype(jnp.float32) + route_b_group.astype(jnp.float32)
    group_probs = jax.nn.softmax(group_logits, axis=-1)
    p_top, g_top = lax.top_k(group_probs, 1)
    p_g, g_idx = p_top[:, 0], g_top[:, 0]
    all_logits = jnp.einsum("nd,gde->nge", h, route_w_expert).astype(jnp.float32) \
        + route_b_expert.astype(jnp.float32)
    in_group = jnp.take_along_axis(all_logits, g_idx[:, None, None], axis=1)[:, 0, :]
    top_vals, top_idx = lax.top_k(in_group, TOP_K)
    weights = (p_g[:, None] * jax.nn.softmax(top_vals, axis=-1)).astype(h.dtype)
    expert_id = g_idx[:, None] * EXPERTS_PER_GROUP + top_idx

    n_assign = n * TOP_K
    eid = expert_id.reshape(-1).astype(jnp.int32)
    tok = jnp.repeat(jnp.arange(n, dtype=jnp.int32), TOP_K)
    wts = weights.reshape(-1)
    order = jnp.argsort(eid)
    s_eid, s_tok, s_w = eid[order], tok[order], wts[order]
    counts = jnp.bincount(eid, length=N_EXPERTS)
    starts = jnp.cumsum(counts) - counts
    padded = ((counts + EXPERT_BLOCK - 1) // EXPERT_BLOCK) * EXPERT_BLOCK
    padded_ends = jnp.cumsum(padded)
    padded_starts = padded_ends - padded
    dest = padded_starts[s_eid] + (jnp.arange(n_assign, dtype=jnp.int32) - starts[s_eid])
    n_rows = -(-n_assign // EXPERT_BLOCK) * EXPERT_BLOCK + N_EXPERTS * EXPERT_BLOCK
    n_blocks = n_rows // EXPERT_BLOCK
    row_tok = jnp.full((n_rows,), n, jnp.int32).at[dest].set(s_tok)
    row_w = jnp.zeros((n_rows,), h.dtype).at[dest].set(s_w)
    block_expert = jnp.minimum(
        jnp.searchsorted(padded_ends, jnp.arange(n_blocks, dtype=jnp.int32) * EXPERT_BLOCK, side="right"),
        N_EXPERTS - 1)
    h_ext = jnp.concatenate([h, jnp.zeros((1, d), h.dtype)], axis=0)
    xb = h_ext[row_tok].reshape(n_blocks, EXPERT_BLOCK, d)

    def expert_block(args):
        x_blk, e = args
        return (jax.nn.silu(x_blk @ w_e_gate[e]) * (x_blk @ w_e_up[e])) @ w_e_down[e]

    yb = lax.map(expert_block, (xb, block_expert)).reshape(n_rows, d)
    y = jnp.zeros((n + 1, d), h.dtype).at[row_tok].add(yb * row_w[:, None])
    return y[:n]


def setup_inputs(seed: int = 0) -> dict:
    key = jax.random.key(seed)
    ks = jax.random.split(key, 32)
    f32 = jnp.float32
    nrm = lambda k, shape, scale: jax.random.normal(k, shape, f32) * scale
    L = DEPTH
    u = jax.random.uniform(ks[14], (L, D_LRU), f32, minval=0.9, maxval=0.999)
    s = u ** (1.0 / LRU_C)
    lru_lambda = jnp.log(s) - jnp.log1p(-s)
    return {
        "x": nrm(ks[0], (BATCH, SEQ, D_MODEL), 1.0),
        "c": nrm(ks[1], (BATCH, D_MODEL), 1.0),
        "ada_w": nrm(ks[2], (L, D_MODEL, 6 * D_MODEL), 0.5 * D_MODEL ** -0.5),
        "ada_b": nrm(ks[3], (L, 6 * D_MODEL), 0.02),
        "norm1_g": 1.0 + nrm(ks[4], (L, D_MODEL), 0.02),
        "w_in": nrm(ks[5], (L, D_MODEL, D_IN), D_MODEL ** -0.5),
        "conv3_w": nrm(ks[6], (L, CONV_WIDTH, D_CONV), CONV_WIDTH ** -0.5),
        "conv4_w": nrm(ks[7], (L, LRU_CONV_WIDTH, D_LRU), LRU_CONV_WIDTH ** -0.5),
        "conv4_b": nrm(ks[8], (L, D_LRU), 0.02),
        "lru_w_a": nrm(ks[9], (L, LRU_HEADS, LRU_HEAD_DIM, LRU_HEAD_DIM), LRU_HEAD_DIM ** -0.5),
        "lru_b_a": nrm(ks[10], (L, D_LRU), 0.02),
        "lru_w_x": nrm(ks[11], (L, LRU_HEADS, LRU_HEAD_DIM, LRU_HEAD_DIM), LRU_HEAD_DIM ** -0.5),
        "lru_b_x": nrm(ks[12], (L, D_LRU), 0.02),
        "lru_lambda": lru_lambda,
        "head_norm_conv_g": 1.0 + nrm(ks[15], (L, D_CONV), 0.02),
        "head_norm_lru_g": 1.0 + nrm(ks[16], (L, D_LRU), 0.02),
        "w_out": nrm(ks[17], (L, D_MIX, D_MODEL), D_MIX ** -0.5),
        "norm2_g": 1.0 + nrm(ks[18], (L, D_MODEL), 0.02),
        "route_w_group": nrm(ks[19], (L, D_MODEL, N_GROUPS), D_MODEL ** -0.5),
        "route_b_group": nrm(ks[20], (L, N_GROUPS), 0.01),
        "route_w_expert": nrm(ks[21], (L, N_GROUPS, D_MODEL, EXPERTS_PER_GROUP), D_MODEL ** -0.5),
        "route_b_expert": nrm(ks[22], (L, N_GROUPS, EXPERTS_PER_GROUP), 0.01),
        "w_e_gate": nrm(ks[23], (L, N_EXPERTS, D_MODEL, D_EXPERT), D_MODEL ** -0.5),
        "w_e_up": nrm(ks[24], (L, N_EXPERTS, D_MODEL, D_EXPERT), D_MODEL ** -0.5),
        "w_e_down": nrm(ks[25], (L, N_EXPERTS, D_EXPERT, D_MODEL), D_EXPERT ** -0.5),
        "final_norm_g": 1.0 + nrm(ks[26], (D_MODEL,), 0.02),
    }


def reference(x, c, ada_w, ada_b, norm1_g, w_in, conv3_w, conv4_w, conv4_b, lru_w_a, lru_b_a,
              lru_w_x, lru_b_x, lru_lambda, head_norm_conv_g, head_norm_lru_g, w_out, norm2_g,
              route_w_group, route_b_group, route_w_expert, route_b_expert,
              w_e_gate, w_e_up, w_e_down, final_norm_g):
    bsz, s, d = x.shape
    for l in range(DEPTH):
        mod = jax.nn.silu(c) @ ada_w[l] + ada_b[l]
        sh1, sc1, g1, sh2, sc2, g2 = jnp.split(mod, 6, axis=-1)
        h = modulated_rmsnorm(x, norm1_g[l], sh1, sc1)
        mix = token_mix(h, w_in[l], conv3_w[l], conv4_w[l], conv4_b[l], lru_w_a[l], lru_b_a[l],
                        lru_w_x[l], lru_b_x[l], lru_lambda[l], head_norm_conv_g[l],
                        head_norm_lru_g[l], w_out[l])
        x = x + g1[:, None, :] * mix
        h = modulated_rmsnorm(x, norm2_g[l], sh2, sc2)
        y = hier_moe(h.reshape(bsz * s, d), route_w_group[l], route_b_group[l], route_w_expert[l],
                     route_b_expert[l], w_e_gate[l], w_e_up[l], w_e_down[l]).reshape(bsz, s, d)
        x = x + g2[:, None, :] * y
    return rmsnorm(x, final_norm_g)
```

```python
import numpy as np
from contextlib import ExitStack
import concourse.bass as bass
import concourse.mybir as mybir
from concourse.bass_utils import run_bass_kernel_spmd

F32 = mybir.dt.float32
BF16 = mybir.dt.bfloat16
I32 = mybir.dt.int32
AF = mybir.ActivationFunctionType
ALU = mybir.AluOpType
AX = mybir.AxisListType

D = 2048
S = 2048
NSEQ = 2
NTOK = NSEQ * S
DIN = 5120
NE = 64
DE = 512
CAP = 512
EPS = 1e-6
NCORES = 8
ARENA_BYTES = 211000
BIG = 1.0e4

ENGS = ("tensor", "vector", "scalar", "gpsimd", "sync")
NDMASEM = 16


class Res:
    __slots__ = ("name", "w", "r")

    def __init__(self, name):
        self.name = name
        self.w = None
        self.r = {}


class Prog:
    def __init__(self, nc, es):
        self.nc = nc
        self.streams = {e: [] for e in ENGS}
        self.cnt = {e: 0 for e in ENGS}
        self.seen = {e: {} for e in ENGS}
        self.esem = {e: es.enter_context(nc.semaphore("cs_" + e)) for e in ENGS if e != "sync"}
        self.dsem = {q: [es.enter_context(nc.semaphore("ds_%s_%d" % (q, i))) for i in range(NDMASEM)]
                     for q in ("sync", "gpsimd")}
        self.dn = {"sync": 0, "gpsimd": 0}

    def _sem(self, key):
        if key[0] == "eng":
            return self.esem[key[1]]
        return self.dsem[key[1]][key[2]]

    def op(self, eng, fn, reads=(), writes=(), dma=False):
        waits = {}
        seen = self.seen[eng]

        def need(tok):
            if tok is None:
                return
            k, v = tok
            if eng == "tensor" and k == ("eng", "tensor"):
                return
            if seen.get(k, 0) >= v:
                return
            if waits.get(k, 0) < v:
                waits[k] = v

        for r in reads:
            need(r.w)
        for w_ in writes:
            need(w_.w)
            for k, v in w_.r.items():
                need((k, v))
        if dma:
            j = self.dn[eng]
            self.dn[eng] += 1
            sidx = j % NDMASEM
            rnd = j // NDMASEM
            k = ("dma", eng, sidx)
            if rnd > 0:
                need((k, 16 * rnd))
            tok = (k, 16 * (rnd + 1))
            inc = (self._sem(k), 16)
        else:
            self.cnt[eng] += 1
            k = ("eng", eng)
            tok = (k, self.cnt[eng])
            inc = (self.esem[eng], 1)
        for k_, v in waits.items():
            seen[k_] = v
        self.streams[eng].append(([(self._sem(k_), v) for k_, v in waits.items()], fn, inc))
        for r in reads:
            if r.r.get(tok[0], 0) < tok[1]:
                r.r[tok[0]] = tok[1]
        for w_ in writes:
            w_.w = tok
            w_.r = {}
        return tok

    def barrier(self):
        toks = []
        for e in ENGS:
            if e != "sync" and self.cnt[e] > 0:
                toks.append((("eng", e), self.cnt[e]))
        for q in ("sync", "gpsimd"):
            n = self.dn[q]
            for sidx in range(min(n, NDMASEM)):
                rounds = (n - 1 - sidx) // NDMASEM + 1
                toks.append((("dma", q, sidx), 16 * rounds))
        for e in ENGS:
            waits = []
            for k, v in toks:
                if e == "tensor" and k == ("eng", "tensor"):
                    continue
                if self.seen[e].get(k, 0) >= v:
                    continue
                self.seen[e][k] = v
                waits.append((self._sem(k), v))
            if waits:
                self.streams[e].append((waits, None, None))

    def emit(self):
        with self.nc.Block() as blk:
            def mk(eng):
                def f(e):
                    for waits, fn, inc in self.streams[eng]:
                        for sem, v in waits:
                            e.wait_ge(sem, v)
                        if fn is not None:
                            ins = fn(e)
                            ins.then_inc(inc[0], inc[1])
                return f
            blk.sync(mk("sync"))
            blk.tensor(mk("tensor"))
            blk.vector(mk("vector"))
            blk.scalar(mk("scalar"))
            blk.gpsimd(mk("gpsimd"))


class Arena:
    def __init__(self, nc):
        self.t = nc.alloc_sbuf_tensor("arena", [128, ARENA_BYTES // 2], BF16)
        self.off = 0

    def alloc(self, shape, dt):
        n = int(np.prod(shape))
        nb = n * (2 if dt == BF16 else 4)
        nb = (nb + 31) // 32 * 32
        assert self.off + nb <= ARENA_BYTES, ("arena overflow", self.off, nb)
        a = self.t[:, self.off // 2:(self.off + nb) // 2]
        self.off += nb
        if dt != BF16:
            a = a.bitcast(dt)
        a = a[:, 0:n]
        if len(shape) == 2:
            a = a.rearrange("p (a b) -> p a b", a=shape[0])
        elif len(shape) == 3:
            a = a.rearrange("p (a b c) -> p a b c", a=shape[0], b=shape[1])
        return a


def build(stop_after="p3", debug=False):
    nc = bass.Bass("TRN2", target_bir_lowering=False)
    es = ExitStack()

    def din(name, shape, dt=F32):
        return nc.dram_tensor(name, shape, dt, kind="ExternalInput").ap()

    x = din("x", [NTOK, D])
    cT = din("cT", [128, 32])
    ada_w = din("ada_w", [D, 6 * D])
    ada_b = din("ada_b", [1, 6 * D])
    norm1_g = din("norm1_g", [1, D])
    norm2_g = din("norm2_g", [1, D])
    final_g = din("final_g", [1, D])
    w_in = din("w_in", [D, DIN])
    w_out = din("w_out", [D, D])
    NP = 56
    smallp = din("smallp", [128, NP])
    lru_w_a = din("lru_w_a", [8, 128, 128])
    lru_w_x = din("lru_w_x", [8, 128, 128])
    wr = din("wr", [D, 72])
    br = din("br", [1, 72])
    w_g = din("w_g", [NE, D, DE])
    w_u = din("w_u", [NE, D, DE])
    w_d = din("w_d", [NE, DE, D])
    out = nc.dram_tensor("out", [NTOK, D], F32, kind="ExternalOutput").ap()
    kind_s = "Internal"
    MOD = nc.dram_tensor("MOD", [2, 6 * D], F32, kind=kind_s).ap()
    X1 = nc.dram_tensor("X1", [NTOK, D], F32, kind=("ExternalOutput" if debug else "Internal")).ap()
    HS = nc.dram_tensor("HS", [NE * CAP + 128, D], BF16, kind=kind_s).ap()
    H2D = nc.dram_tensor("H2D", [NTOK, D], BF16, kind="Internal").ap()
    YS = nc.dram_tensor("YS", [NE * CAP, D], BF16, kind=kind_s).ap()
    if debug:
        DBG = nc.dram_tensor("DBG", [128, 256 + 1152], F32, kind="ExternalOutput").ap()
        DBGY = nc.dram_tensor("DBGY", [16 * 128, S], F32, kind="ExternalOutput").ap()

    P = Prog(nc, es)
    ar = Arena(nc)
    psum = es.enter_context(nc.psum_tensor("psum", [128, 4096], F32))
    psb = psum[:, :].bitcast(BF16)

    def bank(i):
        return psum[:, i * 512:(i + 1) * 512]

    def bankb(i):
        return psb[:, i * 1024:(i + 1) * 1024]

    PB = [Res("bank%d" % i) for i in range(8)]

    def V(fn, r=(), w=()):
        return P.op("vector", fn, r, w)

    def A(fn, r=(), w=()):
        return P.op("scalar", fn, r, w)

    def G(fn, r=(), w=()):
        return P.op("gpsimd", fn, r, w)

    def T(fn, r=(), w=()):
        return P.op("tensor", fn, r, w)

    def DS(fn, r=(), w=()):
        return P.op("sync", fn, r, w, dma=True)

    def DG(fn, r=(), w=()):
        return P.op("gpsimd", fn, r, w, dma=True)

    ident = ar.alloc([128], BF16)
    ltri = ar.alloc([128], BF16)
    onesb = ar.alloc([128], BF16)
    hm64 = ar.alloc([128], BF16)
    hm128 = ar.alloc([128], BF16)
    identf = ar.alloc([128], F32)
    iota_t = ar.alloc([128], F32)
    iota64 = ar.alloc([64], F32)
    sp = ar.alloc([NP], F32)
    klam = ar.alloc([8], F32)
    klam2 = ar.alloc([8], F32)
    ktmp = ar.alloc([8], F32)
    lwa = ar.alloc([8, 128], BF16)
    lwx = ar.alloc([8, 128], BF16)
    tot = ar.alloc([64], F32)
    DEST1 = ar.alloc([32], I32)
    DEST2 = ar.alloc([32], I32)
    WT1 = ar.alloc([32], F32)
    WT2 = ar.alloc([32], F32)
    CONST = Res("const")
    RT = Res("routing_persist")
    TOT = Res("tot")
    C3 = 0
    C4 = 24
    C4B = 56 - 0
    NP2 = 48
    smallp2 = din("smallp2", [128, NP2])
    sp2 = ar.alloc([NP2], F32)

    G(lambda e: e.iota(iota_t, pattern=[[1, 128]], base=0, channel_multiplier=-1,
                       allow_small_or_imprecise_dtypes=True), w=[CONST])
    V(lambda e: e.tensor_single_scalar(ident, iota_t, 0.0, ALU.is_equal), r=[CONST], w=[CONST])
    V(lambda e: e.tensor_single_scalar(identf, iota_t, 0.0, ALU.is_equal), r=[CONST], w=[CONST])
    V(lambda e: e.tensor_single_scalar(ltri, iota_t, 0.0, ALU.is_gt), r=[CONST], w=[CONST])
    V(lambda e: e.memset(onesb, 1.0), w=[CONST])
    V(lambda e: e.memset(hm128, 1.0 / 128), w=[CONST])
    V(lambda e: e.memset(hm64, 0.0), w=[CONST])
    V(lambda e: e.memset(hm64[0:64, 0:64], 1.0 / 64), r=[CONST], w=[CONST])
    V(lambda e: e.memset(hm64[64:128, 64:128], 1.0 / 64), r=[CONST], w=[CONST])
    V(lambda e: e.memset(tot, 0.0), w=[TOT])
    G(lambda e: e.iota(iota64, pattern=[[1, 64]], base=0, channel_multiplier=0,
                       allow_small_or_imprecise_dtypes=True), r=[CONST], w=[CONST])
    DS(lambda e: e.dma_start(out=sp, in_=smallp[:, :]), w=[CONST])
    DS(lambda e: e.dma_start(out=sp2, in_=smallp2[:, :]), r=[CONST], w=[CONST])
    DG(lambda e: e.dma_start(out=lwa, in_=lru_w_a.rearrange("h i j -> i h j")), r=[CONST], w=[CONST])
    DG(lambda e: e.dma_start(out=lwx, in_=lru_w_x.rearrange("h i j -> i h j")), r=[CONST], w=[CONST])
    A(lambda e: e.activation(out=ktmp, in_=sp2[:, 24:32], func=AF.Exp, scale=-1.0), r=[CONST], w=[CONST])
    A(lambda e: e.activation(out=ktmp, in_=ktmp, func=AF.Ln, bias=1.0, scale=1.0), r=[CONST], w=[CONST])
    V(lambda e: e.tensor_scalar(klam, ktmp, -8.0, None, ALU.mult), r=[CONST], w=[CONST])
    V(lambda e: e.tensor_scalar(klam2, ktmp, -16.0, None, ALU.mult), r=[CONST], w=[CONST])
    P.barrier()
    CONST.w = None
    CONST.r = {}
    const_end = ar.off

    cts = ar.alloc([32], F32)
    siluT = ar.alloc([16, 2], BF16)
    adaw = [ar.alloc([16, 512], BF16) for _ in range(3)]
    abt = [ar.alloc([512], F32) for _ in range(2)]
    modrow = [ar.alloc([512], F32) for _ in range(2)]
    R_adaw = [Res("adaw%d" % i) for i in range(3)]
    R_abt = [Res("abt%d" % i) for i in range(2)]
    R_mr = [Res("mr%d" % i) for i in range(2)]
    R_silu = Res("silu")
    DS(lambda e: e.dma_start(out=cts, in_=cT[:, :]), w=[R_silu])
    A(lambda e: e.activation(out=siluT.rearrange("p a b -> p (a b)"), in_=cts, func=AF.Silu), r=[R_silu], w=[R_silu])
    for j in range(24):
        sl = j % 3
        s2 = j % 2
        DG(lambda e, j=j, sl=sl: e.dma_start(
            out=adaw[sl], in_=ada_w[:, j * 512:(j + 1) * 512].rearrange("(k p) n -> p k n", p=128)),
            w=[R_adaw[sl]])
        DS(lambda e, j=j, s2=s2: e.dma_start(
            out=abt[s2][0:2, :], in_=ada_b[0:1, j * 512:(j + 1) * 512].to_broadcast([2, 512])), w=[R_abt[s2]])

        def mm0(e, sl=sl, s2=s2):
            ins = None
            for k in range(16):
                ins = e.matmul(bank(s2)[0:2, :], siluT[:, k, :], adaw[sl][:, k, :], start=(k == 0), stop=(k == 15))
            return ins
        T(mm0, r=[R_silu, R_adaw[sl]], w=[PB[s2]])
        V(lambda e, s2=s2: e.tensor_tensor(modrow[s2][0:2, :], bank(s2)[0:2, :], abt[s2][0:2, :], ALU.add),
          r=[PB[s2], R_abt[s2]], w=[R_mr[s2]])
        DS(lambda e, j=j, s2=s2: e.dma_start(out=MOD[0:2, j * 512:(j + 1) * 512], in_=modrow[s2][0:2, :]),
           r=[R_mr[s2]])
    P.barrier()
    if stop_after == "p0":
        P.emit()
        return nc

    ar.off = const_end
    R1 = ar.alloc([16, S], BF16)
    ynT = ar.alloc([16, S], BF16)
    stage_base = ar.off

    def emit_skewed(tiles, order=None):
        depth = max(len(t_) for t_ in tiles)
        for step in range(len(tiles) + depth - 1):
            for k in (order if order is not None else list(range(1, depth)) + [0]):
                j = step - k
                if 0 <= j < len(tiles) and k < len(tiles[j]):
                    tiles[j][k]()

    for s in range(NSEQ):
        ar.off = stage_base
        Abc = ar.alloc([D], F32)
        Bbc = ar.alloc([D], F32)
        xtb = [ar.alloc([D], F32) for _ in range(2)]
        t1 = ar.alloc([D], F32)
        hb = [ar.alloc([D], BF16) for _ in range(2)]
        ssq = [ar.alloc([1], F32) for _ in range(2)]
        srt = [ar.alloc([1], F32) for _ in range(2)]
        rstd = [ar.alloc([1], F32) for _ in range(2)]
        R_A, R_B = Res("Abc"), Res("Bbc")
        R_xt = [Res("xt0"), Res("xt1")]
        R_t1 = Res("t1")
        R_hb = [Res("hb0"), Res("hb1")]
        R_st = [Res("st0"), Res("st1")]
        R_hT = [Res("hT%d" % t) for t in range(16)]
        hT = R1
        DS(lambda e, s=s: e.dma_start(out=Abc, in_=MOD[s:s + 1, 2048:4096].to_broadcast([128, D])), w=[R_A])
        DS(lambda e: e.dma_start(out=t1, in_=norm1_g[0:1, :].to_broadcast([128, D])), w=[R_t1])
        DS(lambda e, s=s: e.dma_start(out=Bbc, in_=MOD[s:s + 1, 0:2048].to_broadcast([128, D])), w=[R_B])
        V(lambda e: e.scalar_tensor_tensor(Abc, Abc, 1.0, t1, ALU.add, ALU.mult), r=[R_t1, R_A], w=[R_A])

        def load_x(t, s=s):
            b = t % 2
            DS(lambda e: e.dma_start(out=xtb[b], in_=x[s * S + t * 128:s * S + (t + 1) * 128, :]), w=[R_xt[b]])
        t1b = [t1, ar.alloc([D], F32)]
        R_t1b = [R_t1, Res("t1b")]

        def make_tileA(t):
            b = t % 2

            def a0_():
                if t + 1 < 16:
                    load_x(t + 1)
                V(lambda e: e.memset(ssq[b], 0.0), w=[R_st[b]])
                A(lambda e: e.activation(out=hb[b], in_=xtb[b], func=AF.Square, accum_out=ssq[b]),
                  r=[R_xt[b]], w=[R_hb[b], R_st[b]])
                A(lambda e: e.activation(out=srt[b], in_=ssq[b], func=AF.Sqrt, bias=EPS, scale=1.0 / D),
                  r=[R_st[b]], w=[R_st[b]])
                V(lambda e: e.reciprocal(rstd[b], srt[b]), r=[R_st[b]], w=[R_st[b]])
                V(lambda e: e.scalar_tensor_tensor(t1b[b], xtb[b], rstd[b], Abc, ALU.mult, ALU.mult),
                  r=[R_xt[b], R_st[b], R_A], w=[R_t1b[b]])
                G(lambda e: e.tensor_tensor(hb[b], t1b[b], Bbc, ALU.add), r=[R_t1b[b], R_B], w=[R_hb[b]])

            def a1_():
                def trA(e):
                    ins = None
                    for k in range(16):
                        bk = 2 * b + k // 8
                        ins = e.transpose(bankb(bk)[:, (k % 8) * 128:(k % 8 + 1) * 128], hb[b][:, k * 128:(k + 1) * 128], ident)
                    return ins
                T(trA, r=[R_hb[b], CONST], w=[PB[2 * b], PB[2 * b + 1]])
                A(lambda e: e.activation(out=hT[:, 0:8, t * 128:(t + 1) * 128],
                                         in_=bankb(2 * b).rearrange("p (a b) -> p a b", a=8), func=AF.Copy),
                  r=[PB[2 * b]], w=[R_hT[t]])
                V(lambda e: e.tensor_copy(hT[:, 8:16, t * 128:(t + 1) * 128],
                                          bankb(2 * b + 1).rearrange("p (a b) -> p a b", a=8)),
                  r=[PB[2 * b + 1]], w=[R_hT[t]])
            return [a0_, a1_]
        load_x(0)
        emit_skewed([make_tileA(t) for t in range(16)], order=(0, 1))
        P.barrier()

        ar.off = stage_base
        NWS = 6
        win = [ar.alloc([16, 128], BF16) for _ in range(NWS)]
        R_win = [Res("win%d" % i) for i in range(NWS)]
        cxb = ar.alloc([3 + S], F32)
        xrb = cxb
        R_cx = [Res("cx%d" % n) for n in range(4)]
        R_xr = R_cx

        class TS:
            pass
        tsets = []
        for q in range(2):
            t_ = TS()
            for nm, dt_ in (("cs", F32), ("acc", F32), ("ya", F32), ("sqb", BF16), ("srtB", F32), ("xbb", BF16),
                            ("ii", F32), ("aa", F32), ("ml", F32), ("h", F32)):
                setattr(t_, nm, ar.alloc([512], dt_))
                setattr(t_, "R_" + nm, Res("%s%d" % (nm, q)))
            t_.rr, t_.R_rr = t_.srtB, t_.R_srtB
            t_.uu, t_.R_uu = t_.ml, t_.R_ml
            tsets.append(t_)
        R_yn = [Res("yn%d" % c) for c in range(16)]
        V(lambda e: e.memset(cxb[:, 0:3], 0.0), w=[R_cx[0]])
        def load_w(c):
            if c < 8:
                cols = [c * 128, 1024 + c * 128, 2048 + c * 128]
            else:
                cols = [3072 + (c - 8) * 128, 4096 + (c - 8) * 128]
            slots = []
            for col in cols:
                sl = wstate[0] % NWS
                wstate[0] += 1
                DG(lambda e, sl=sl, col=col: e.dma_start(
                    out=win[sl], in_=w_in[:, col:col + 128].rearrange("(k p) n -> p k n", p=128)), w=[R_win[sl]])
                slots.append(sl)
            wslots[c] = slots

        def make_tile(c, n, par):
            st = par * 3
            ts = tsets[par]
            tp = tsets[1 - par]
            tk = slice(n * 512, (n + 1) * 512)
            c0 = 3 + n * 512

            def mm_main():
                if n == 1 and c + 1 < 16:
                    load_w(c + 1)
                for mi, sl in enumerate(wslots[c]):
                    def mmB(e, sl=sl, bk=st + mi):
                        ins = None
                        for k in range(16):
                            ins = e.matmul(bank(bk), win[sl][:, k, :], hT[:, k, tk], start=(k == 0), stop=(k == 15))
                        return ins
                    T(mmB, r=[R_win[sl]], w=[PB[st + mi]])
            if c < 8:
                pBk, pCk, pXk = st, st + 1, st + 2
                aux = 6 + (n % 2)
                rcx = [R_cx[n]] + ([R_cx[n - 1]] if n > 0 else [])

                def q0():
                    mm_main()
                    A(lambda e: e.activation(out=ts.cs, in_=bank(pCk), func=AF.Copy), r=[PB[pCk]], w=[ts.R_cs])
                    V(lambda e: e.tensor_tensor(cxb[:, c0:c0 + 512], ts.cs, bank(pXk), ALU.mult),
                      r=[ts.R_cs, PB[pXk]], w=[R_cx[n]])
                    V(lambda e: e.tensor_scalar(ts.acc, cxb[:, c0:c0 + 512], sp[:, c * 3 + 2:c * 3 + 3], None, ALU.mult),
                      r=rcx, w=[ts.R_acc])
                    for dk in (1, 2):
                        V(lambda e, dk=dk: e.scalar_tensor_tensor(
                            ts.acc, cxb[:, c0 - dk:c0 - dk + 512], sp[:, c * 3 + 2 - dk:c * 3 + 3 - dk], ts.acc, ALU.mult, ALU.add),
                          r=rcx + [ts.R_acc], w=[ts.R_acc])
                    V(lambda e: e.tensor_tensor(ts.ya, ts.acc, bank(pBk), ALU.mult), r=[ts.R_acc, PB[pBk]], w=[ts.R_ya])
                    A(lambda e: e.activation(out=ts.sqb, in_=ts.ya, func=AF.Square), r=[ts.R_ya], w=[ts.R_sqb])

                def q1():
                    T(lambda e: e.matmul(bank(aux), hm64, ts.sqb, start=True, stop=True), r=[ts.R_sqb], w=[PB[aux]])
                    A(lambda e: e.activation(out=ts.srtB, in_=bank(aux), func=AF.Sqrt, bias=EPS, scale=1.0),
                      r=[PB[aux]], w=[ts.R_srtB])
                    V(lambda e: e.reciprocal(ts.srtB, ts.srtB), r=[ts.R_srtB], w=[ts.R_srtB])
                    V(lambda e: e.scalar_tensor_tensor(ynT[:, c, tk], ts.ya, sp2[:, 32 + c:33 + c], ts.srtB, ALU.mult, ALU.mult),
                      r=[ts.R_ya, ts.R_srtB], w=[R_yn[c]])
                return [q0, q1]
            h8 = c - 8
            pGk, pXk, pSk = st, st + 1, st + 2
            rxr = [R_xr[n]] + ([R_xr[n - 1]] if n > 0 else [])

            def p0():
                mm_main()
                A(lambda e: e.activation(out=ts.cs, in_=bank(pGk), func=AF.Gelu_apprx_tanh), r=[PB[pGk]], w=[ts.R_cs])
                A(lambda e: e.activation(out=xrb[:, c0:c0 + 512], in_=bank(pXk), func=AF.Copy), r=[PB[pXk]], w=[R_xr[n]])
                V(lambda e: e.tensor_scalar(ts.acc, xrb[:, c0:c0 + 512], sp[:, 24 + h8 * 4 + 3:24 + h8 * 4 + 4],
                                            sp2[:, h8:h8 + 1], ALU.mult, ALU.add), r=rxr, w=[ts.R_acc])
                for dk in (1, 2, 3):
                    V(lambda e, dk=dk: e.scalar_tensor_tensor(
                        ts.acc, xrb[:, c0 - dk:c0 - dk + 512], sp[:, 24 + h8 * 4 + 3 - dk:24 + h8 * 4 + 4 - dk], ts.acc, ALU.mult, ALU.add),
                      r=rxr + [ts.R_acc], w=[ts.R_acc])
                G(lambda e: e.tensor_copy(ts.xbb, ts.acc), r=[ts.R_acc], w=[ts.R_xbb])

            def p1():
                T(lambda e: e.matmul(bank(6), lwa[:, h8, :], ts.xbb, start=True, stop=True), r=[ts.R_xbb, CONST], w=[PB[6]])
                T(lambda e: e.matmul(bank(7), lwx[:, h8, :], ts.xbb, start=True, stop=True), r=[ts.R_xbb, CONST], w=[PB[7]])
                A(lambda e: e.activation(out=ts.rr, in_=bank(6), func=AF.Sigmoid, bias=sp2[:, 8 + h8:9 + h8], scale=1.0),
                  r=[PB[6]], w=[ts.R_rr])
                A(lambda e: e.activation(out=ts.ii, in_=bank(7), func=AF.Sigmoid, bias=sp2[:, 16 + h8:17 + h8], scale=1.0),
                  r=[PB[7]], w=[ts.R_ii])
                A(lambda e: e.activation(out=ts.aa, in_=ts.rr, func=AF.Exp, scale=klam[:, h8:h8 + 1]), r=[ts.R_rr], w=[ts.R_aa])
                A(lambda e: e.activation(out=ts.ml, in_=ts.rr, func=AF.Exp, scale=klam2[:, h8:h8 + 1]), r=[ts.R_rr], w=[ts.R_ml])
                A(lambda e: e.activation(out=ts.ml, in_=ts.ml, func=AF.Sqrt, bias=1.0, scale=-1.0), r=[ts.R_ml], w=[ts.R_ml])
                V(lambda e: e.tensor_tensor(ts.uu, ts.ml, ts.ii, ALU.mult), r=[ts.R_ml, ts.R_ii], w=[ts.R_uu])
                V(lambda e: e.tensor_tensor(ts.uu, ts.uu, ts.acc, ALU.mult), r=[ts.R_uu, ts.R_acc], w=[ts.R_uu])
                if n == 0:
                    V(lambda e: e.tensor_tensor_scan(ts.h, ts.aa, ts.uu, 0.0, ALU.mult, ALU.add),
                      r=[ts.R_aa, ts.R_uu], w=[ts.R_h])
                else:
                    V(lambda e: e.tensor_tensor_scan(ts.h, ts.aa, ts.uu, tp.h[:, 511:512], ALU.mult, ALU.add),
                      r=[ts.R_aa, ts.R_uu, tp.R_h], w=[ts.R_h])
                V(lambda e: e.tensor_tensor(ts.ya, ts.h, ts.cs, ALU.mult), r=[ts.R_h, ts.R_cs], w=[ts.R_ya])
                A(lambda e: e.activation(out=ts.sqb, in_=ts.ya, func=AF.Square), r=[ts.R_ya], w=[ts.R_sqb])

            def p2():
                T(lambda e: e.matmul(bank(pSk), hm128, ts.sqb, start=True, stop=True), r=[ts.R_sqb], w=[PB[pSk]])
                A(lambda e: e.activation(out=ts.srtB, in_=bank(pSk), func=AF.Sqrt, bias=EPS, scale=1.0),
                  r=[PB[pSk]], w=[ts.R_srtB])
                V(lambda e: e.reciprocal(ts.srtB, ts.srtB), r=[ts.R_srtB], w=[ts.R_srtB])
                V(lambda e: e.scalar_tensor_tensor(ynT[:, c, tk], ts.ya, sp2[:, 40 + h8:41 + h8], ts.srtB, ALU.mult, ALU.mult),
                  r=[ts.R_ya, ts.R_srtB], w=[R_yn[c]])
            return [p0, p1, p2]

        wstate = [0]
        wslots = {}
        load_w(0)
        tilesB = []
        ntc = 0
        for c in range(16):
            for n in range(4):
                tilesB.append(make_tile(c, n, ntc % 2))
                ntc += 1
        emit_skewed(tilesB, order=(0, 1, 2))
        P.barrier()
        if debug and s == 0:
            for c in range(16):
                for n in range(4):
                    V(lambda e, c=c, n=n: e.tensor_copy(tsets[0].acc, ynT[:, c, n * 512:(n + 1) * 512]), w=[tsets[0].R_acc])
                    DS(lambda e, c=c, n=n: e.dma_start(out=DBGY[c * 128:(c + 1) * 128, n * 512:(n + 1) * 512], in_=tsets[0].acc), r=[tsets[0].R_acc])
            P.barrier()

        ar.off = stage_base
        wo = R1
        Abc = ar.alloc([D], F32)
        Bbc = ar.alloc([D], F32)
        lgall = ar.alloc([16, 72], F32)
        routing_base = ar.off
        G1 = ar.alloc([D], F32)
        xtC = [ar.alloc([D], F32) for _ in range(2)]
        h2f = ar.alloc([D], F32)
        h2b = ar.alloc([D], BF16)
        h2T = ar.alloc([16, 128], F32)
        wrt = ar.alloc([16, 72], F32)
        brbc = ar.alloc([72], F32)
        R_wr = Res("wr")
        DS(lambda e: e.dma_start(out=wrt, in_=wr.rearrange("(k p) n -> p k n", p=128)), w=[R_wr])
        DS(lambda e: e.dma_start(out=brbc, in_=br[0:1, :].to_broadcast([128, 72])), r=[R_wr], w=[R_wr])
        R_A, R_B, R_G1 = Res("A2"), Res("B2"), Res("G1")
        R_xtC = [Res("xtC0"), Res("xtC1")]
        R_h2f, R_h2b, R_h2T = Res("h2f"), Res("h2b"), Res("h2T")
        R_pmg2 = [Res("pmg0")] * 2
        R_wo = [Res("wo%d" % i) for i in range(4)]
        R_sm = Res("small")
        R_n2 = Res("n2small")

        def sm(n, dt=F32):
            return ar.alloc([n], dt)
        ssq2, srt2, rstd2 = sm(1), sm(1), sm(1)
        R_lg = Res('lgall')

        for q in range(4):
            DG(lambda e, q=q: e.dma_start(out=wo[:, q * 4:(q + 1) * 4, :],
                                          in_=w_out[q * 512:(q + 1) * 512, :].rearrange("(k p) n -> p k n", p=128)),
               w=[R_wo[q]])
        DS(lambda e, s=s: e.dma_start(out=Abc, in_=MOD[s:s + 1, 8192:10240].to_broadcast([128, D])), w=[R_A])
        DS(lambda e: e.dma_start(out=h2f, in_=norm2_g[0:1, :].to_broadcast([128, D])), w=[R_h2f])
        DS(lambda e, s=s: e.dma_start(out=Bbc, in_=MOD[s:s + 1, 6144:8192].to_broadcast([128, D])), w=[R_B])
        DS(lambda e, s=s: e.dma_start(out=G1, in_=MOD[s:s + 1, 4096:6144].to_broadcast([128, D])), w=[R_G1])
        V(lambda e: e.scalar_tensor_tensor(Abc, Abc, 1.0, h2f, ALU.add, ALU.mult), r=[R_h2f, R_A], w=[R_A])

        def make_tileC(t):
            col = s * 16 + t
            tok0 = s * S + t * 128
            xt = xtC[t % 2]
            R_xt = R_xtC[t % 2]

            def c0():
                DS(lambda e, tok0=tok0: e.dma_start(out=xt, in_=x[tok0:tok0 + 128, :]), w=[R_xt])
                for nsl in range(4):
                    def mmC(e, nsl=nsl, t=t):
                        ins = None
                        for k in range(16):
                            ins = e.matmul(bank(nsl), ynT[:, k, t * 128:(t + 1) * 128], wo[:, k, nsl * 512:(nsl + 1) * 512],
                                           start=(k == 0), stop=(k == 15))
                        return ins
                    T(mmC, r=R_wo, w=[PB[nsl]])
                    V(lambda e, nsl=nsl: e.tensor_tensor(bank(nsl), bank(nsl), G1[:, nsl * 512:(nsl + 1) * 512], ALU.mult),
                      r=[PB[nsl], R_G1], w=[PB[nsl]])
                    V(lambda e, nsl=nsl: e.tensor_tensor(xt[:, nsl * 512:(nsl + 1) * 512], xt[:, nsl * 512:(nsl + 1) * 512], bank(nsl), ALU.add),
                      r=[PB[nsl], R_xt], w=[R_xt])
                DS(lambda e, tok0=tok0: e.dma_start(out=X1[tok0:tok0 + 128, :], in_=xt), r=[R_xt])

            def c1():
                V(lambda e: e.memset(ssq2, 0.0), w=[R_n2])
                A(lambda e: e.activation(out=h2T.rearrange("p a b -> p (a b)"), in_=xt, func=AF.Square, accum_out=ssq2),
                  r=[R_xt, R_n2], w=[R_h2T, R_n2])
                A(lambda e: e.activation(out=srt2, in_=ssq2, func=AF.Sqrt, bias=EPS, scale=1.0 / D), r=[R_n2], w=[R_n2])
                V(lambda e: e.reciprocal(rstd2, srt2), r=[R_n2], w=[R_n2])
                V(lambda e: e.scalar_tensor_tensor(h2f, xt, rstd2, Abc, ALU.mult, ALU.mult), r=[R_xt, R_n2, R_A], w=[R_h2f])
                G(lambda e: e.tensor_tensor(h2f, h2f, Bbc, ALU.add), r=[R_h2f, R_B], w=[R_h2f])
                A(lambda e: e.activation(out=h2b, in_=h2f, func=AF.Copy), r=[R_h2f], w=[R_h2b])
                DS(lambda e: e.dma_start(out=H2D[tok0:tok0 + 128, :], in_=h2b), r=[R_h2b])
                for half in range(2):
                    def trC(e, half=half):
                        ins = None
                        for kk in range(8):
                            k = half * 8 + kk
                            ins = e.transpose(psum[:, (4 + half * 2) * 512 + kk * 128:(4 + half * 2) * 512 + (kk + 1) * 128],
                                              h2f[:, k * 128:(k + 1) * 128], identf)
                        return ins
                    T(trC, r=[R_h2f, CONST], w=[PB[4 + half * 2], PB[5 + half * 2]])
                    for q2 in range(2):
                        bkq = 4 + half * 2 + q2
                        srcq = bank(bkq).rearrange("p (a b) -> p a b", a=4)
                        k0 = half * 8 + q2 * 4
                        A(lambda e, srcq=srcq, k0=k0: e.activation(out=h2T[:, k0:k0 + 4, :], in_=srcq, func=AF.Copy),
                          r=[PB[bkq]], w=[R_h2T])

                def mmL(e):
                    ins = None
                    for k in range(16):
                        ins = e.matmul(bank(4)[:, 0:72], h2T[:, k, :], wrt[:, k, :], start=(k == 0), stop=(k == 15))
                    return ins
                T(mmL, r=[R_h2T, R_wr], w=[PB[4]])

                V(lambda e: e.tensor_tensor(lgall[:, t, :], bank(4)[:, 0:72], brbc, ALU.add), r=[PB[4], R_wr], w=[R_lg])

            return [c0, c1]

        tilesC = [make_tileC(t) for t in range(16)]
        emit_skewed(tilesC, order=(0, 1))
        P.barrier()
        ar.off = routing_base
        h2all = R1
        DS(lambda e, s=s: e.dma_start(out=h2all[:, 0:8, :], in_=H2D[s * S:s * S + 1024, :].rearrange("(t p) d -> p t d", p=128)), w=[R_h2b])
        DS(lambda e, s=s: e.dma_start(out=h2all[:, 8:16, :], in_=H2D[s * S + 1024:(s + 1) * S, :].rearrange("(t p) d -> p t d", p=128)), w=[R_xtC[0]])
        R_rt = Res("rt")
        rs = [R_rt]

        def a16(dt=F32):
            return ar.alloc([16], dt)

        def a168():
            return ar.alloc([16, 8], F32)

        def a1664(dt=F32):
            return ar.alloc([16, 64], dt)
        gmax, sumg, pg, v1, v2, dv, ev, den, w1s, w2s = [a16() for _ in range(10)]
        pp, ee_, ov, nov, d1, dsc = [a16() for _ in range(6)]
        dsci = [a16(I32), a16(I32)]
        tmpg, mg, eg, pen = a168(), a168(), a168(), a168()
        msk, oh1, oh2, tmpL, base = a1664(), a1664(), a1664(), a1664(), a1664()
        abf = a1664(BF16)
        lg_g = lgall[:, :, 0:8]
        lg_e4 = lgall[:, :, 8:72].rearrange("p t (g e) -> p t g e", g=8)
        msk4 = msk.rearrange("p t (g e) -> p t g e", g=8)
        fl = lambda a_: a_.rearrange("p a b -> p (a b)")
        bc8 = lambda a_: a_.unsqueeze(2).to_broadcast([128, 16, 8])
        bc64 = lambda a_: a_.unsqueeze(2).to_broadcast([128, 16, 64])
        V(lambda e: e.reduce_max(gmax, lg_g, AX.X), r=[R_lg], w=rs)
        V(lambda e: e.tensor_tensor(mg, lg_g, bc8(gmax), ALU.is_ge), r=rs, w=rs)
        V(lambda e: e.tensor_tensor(tmpg, lg_g, bc8(gmax), ALU.subtract), r=rs, w=rs)
        A(lambda e: e.activation(out=fl(eg), in_=fl(tmpg), func=AF.Exp), r=rs, w=rs)
        V(lambda e: e.reduce_sum(sumg, eg, AX.X), r=rs, w=rs)
        V(lambda e: e.reciprocal(pg, sumg), r=rs, w=rs)
        V(lambda e: e.tensor_scalar(fl(pen), fl(mg), BIG, -BIG, ALU.mult, ALU.add), r=rs, w=rs)
        for g in range(8):
            V(lambda e, g=g: e.tensor_tensor(msk[:, :, g * 8:(g + 1) * 8], lgall[:, :, 8 + g * 8:16 + g * 8],
                                             pen[:, :, g:g + 1].to_broadcast([128, 16, 8]), ALU.add), r=rs, w=rs)
        V(lambda e: e.reduce_max(v1, msk, AX.X), r=rs, w=rs)
        V(lambda e: e.tensor_tensor(oh1, msk, bc64(v1), ALU.is_equal), r=rs, w=rs)
        V(lambda e: e.scalar_tensor_tensor(fl(msk), fl(oh1), -BIG, fl(msk), ALU.mult, ALU.add), r=rs, w=rs)
        V(lambda e: e.reduce_max(v2, msk, AX.X), r=rs, w=rs)
        V(lambda e: e.tensor_tensor(oh2, msk, bc64(v2), ALU.is_equal), r=rs, w=rs)
        V(lambda e: e.tensor_tensor(dv, v2, v1, ALU.subtract), r=rs, w=rs)
        A(lambda e: e.activation(out=ev, in_=dv, func=AF.Exp), r=rs, w=rs)
        V(lambda e: e.tensor_scalar(den, ev, 1.0, None, ALU.add), r=rs, w=rs)
        V(lambda e: e.reciprocal(w1s, den), r=rs, w=rs)
        V(lambda e: e.tensor_tensor(w2s, ev, w1s, ALU.mult), r=rs, w=rs)
        V(lambda e: e.tensor_tensor(w1s, w1s, pg, ALU.mult), r=rs, w=rs)
        V(lambda e: e.tensor_tensor(w2s, w2s, pg, ALU.mult), r=rs, w=rs)
        V(lambda e: e.tensor_tensor(fl(abf), fl(oh1), fl(oh2), ALU.add), r=rs, w=rs)

        def mmPB(e):
            ins = None
            for half in range(2):
                e.matmul(bank(half), ltri, fl(abf)[:, half * 512:(half + 1) * 512], start=True, stop=True)
                ins = e.matmul(bank(2 + half), onesb, fl(abf)[:, half * 512:(half + 1) * 512], start=True, stop=True)
            return ins
        T(mmPB, r=[R_rt, CONST], w=[PB[0], PB[1], PB[2], PB[3]])
        V(lambda e: e.tensor_copy(base[:, 0, :], tot), r=[TOT, R_rt], w=rs)
        for j in range(1, 16):
            jj = j - 1
            V(lambda e, j=j, jj=jj: e.tensor_tensor(base[:, j, :], base[:, jj, :], bank(2 + jj // 8)[:, (jj % 8) * 64:(jj % 8 + 1) * 64], ALU.add),
              r=rs + [PB[2], PB[3]], w=rs)
        V(lambda e: e.tensor_tensor(tot, base[:, 15, :], bank(3)[:, 7 * 64:8 * 64], ALU.add), r=rs + [PB[3]], w=[TOT])
        posf = msk
        for half in range(2):
            V(lambda e, half=half: e.tensor_tensor(fl(posf)[:, half * 512:(half + 1) * 512], bank(half), fl(base)[:, half * 512:(half + 1) * 512], ALU.add),
              r=rs + [PB[half]], w=rs)
        c0_ = s * 16
        for (oh, wsl, DST, WTT, ji) in ((oh1, w1s, DEST1, WT1, 0), (oh2, w2s, DEST2, WT2, 1)):
            V(lambda e, oh=oh: e.tensor_tensor(fl(tmpL), fl(oh), fl(posf), ALU.mult), r=rs, w=rs)
            V(lambda e: e.reduce_sum(pp, tmpL, AX.X), r=rs, w=rs)
            V(lambda e, oh=oh: e.tensor_tensor(tmpL, oh, iota64.unsqueeze(1).to_broadcast([128, 16, 64]), ALU.mult), r=rs + [CONST], w=rs)
            V(lambda e: e.reduce_sum(ee_, tmpL, AX.X), r=rs, w=rs)
            V(lambda e: e.tensor_scalar(ov, pp, float(CAP), None, ALU.is_ge), r=rs, w=rs)
            V(lambda e: e.tensor_scalar(nov, ov, -1.0, 1.0, ALU.mult, ALU.add), r=rs, w=rs)
            V(lambda e: e.scalar_tensor_tensor(d1, ee_, float(CAP), pp, ALU.mult, ALU.add), r=rs, w=rs)
            V(lambda e: e.tensor_tensor(d1, d1, nov, ALU.mult), r=rs, w=rs)
            V(lambda e: e.scalar_tensor_tensor(dsc, ov, float(NE * CAP), d1, ALU.mult, ALU.add), r=rs, w=rs)
            V(lambda e, ji=ji: e.tensor_copy(dsci[ji], dsc), r=rs, w=rs)
            V(lambda e, DST=DST, c0_=c0_: e.tensor_copy(DST[:, c0_:c0_ + 16], d1), r=rs, w=[RT, R_rt])
            V(lambda e, WTT=WTT, wsl=wsl, c0_=c0_: e.tensor_tensor(WTT[:, c0_:c0_ + 16], wsl, nov, ALU.mult), r=rs, w=[RT, R_rt])
            for t in range(16):
                DG(lambda e, ji=ji, t=t: e.indirect_dma_start(
                    out=HS[:, :], out_offset=bass.IndirectOffsetOnAxis(ap=dsci[ji][:, t:t + 1], axis=0),
                    in_=h2all[:, t, :], in_offset=None),
                   r=[R_rt, R_h2b, R_xtC[0]])
        P.barrier()
    if debug:
        dbt = ar.alloc([128], F32)
        R_dbt = Res("dbt")
        V(lambda e: e.tensor_copy(dbt[:, 0:32], DEST1), r=[RT], w=[R_dbt])
        V(lambda e: e.tensor_copy(dbt[:, 32:64], DEST2), r=[RT, R_dbt], w=[R_dbt])
        V(lambda e: e.tensor_copy(dbt[:, 64:96], WT1), r=[RT, R_dbt], w=[R_dbt])
        V(lambda e: e.tensor_copy(dbt[:, 96:128], WT2), r=[RT, R_dbt], w=[R_dbt])
        DS(lambda e: e.dma_start(out=DBG[:, 0:128], in_=dbt), r=[R_dbt])
        dbt2 = ar.alloc([128], F32)
        V(lambda e: e.tensor_copy(dbt2[:, 0:16], gmax), w=[R_dbt])
        V(lambda e: e.tensor_copy(dbt2[:, 16:32], v1), r=[R_dbt], w=[R_dbt])
        V(lambda e: e.tensor_copy(dbt2[:, 32:40], lgall[:, 0, 0:8]), r=[R_dbt], w=[R_dbt])
        V(lambda e: e.tensor_copy(dbt2[:, 40:48], mg[:, 0, :]), r=[R_dbt], w=[R_dbt])
        V(lambda e: e.tensor_copy(dbt2[:, 48:56], pen[:, 0, :]), r=[R_dbt], w=[R_dbt])
        V(lambda e: e.tensor_copy(dbt2[:, 56:64], lgall[:, 5, 0:8]), r=[R_dbt], w=[R_dbt])
        V(lambda e: e.tensor_copy(dbt2[:, 64:128], lgall[:, 0, 8:72]), r=[R_dbt], w=[R_dbt])
        DS(lambda e: e.dma_start(out=DBG[:, 128:256], in_=dbt2), r=[R_dbt])
        DS(lambda e: e.dma_start(out=DBG[:, 256:256 + 1152], in_=lgall.rearrange("p a b -> p (a b)")), r=[R_dbt])
        P.barrier()
    if stop_after == "p1":
        P.emit()
        return nc

    ar.off = const_end
    xe = [ar.alloc([4, D], BF16) for _ in range(2)]
    wgb = [ar.alloc([16, DE], BF16) for _ in range(2)]
    wub = [ar.alloc([16, DE], BF16) for _ in range(2)]
    wdb = [ar.alloc([4, D], BF16) for _ in range(2)]
    hTe2 = [ar.alloc([16, CAP], BF16) for _ in range(2)]
    aT = ar.alloc([4, CAP], BF16)
    sg = [ar.alloc([512], F32) for _ in range(2)]
    ye = [ar.alloc([D], BF16) for _ in range(2)]
    R_xe = [Res("xe0"), Res("xe1")]
    R_wg = [Res("wg0"), Res("wg1")]
    R_wu = [Res("wu0"), Res("wu1")]
    R_wd = [Res("wd0"), Res("wd1")]
    R_hTe2 = [[Res("hTe%d_%d" % (q, k)) for k in range(16)] for q in range(2)]
    R_aT = [Res("aT%d" % m) for m in range(4)]
    R_sg = [Res("sg0"), Res("sg1")]
    R_ye = [Res("ye0"), Res("ye1")]

    def load_xe(ex_):
        b_ = ex_ % 2
        DS(lambda e: e.dma_start(out=xe[b_], in_=HS[ex_ * CAP:(ex_ + 1) * CAP, :].rearrange("(r p) d -> p r d", p=128)),
           w=[R_xe[b_]])

    def load_we(ex_):
        b_ = ex_ % 2
        DG(lambda e: e.dma_start(out=wgb[b_], in_=w_g[ex_].rearrange("(k p) n -> p k n", p=128)), w=[R_wg[b_]])
        DG(lambda e: e.dma_start(out=wub[b_], in_=w_u[ex_].rearrange("(k p) n -> p k n", p=128)), w=[R_wu[b_]])
        DG(lambda e: e.dma_start(out=wdb[b_], in_=w_d[ex_].rearrange("(k p) n -> p k n", p=128)), w=[R_wd[b_]])

    def tr_chunk(ex_, k_):
        b_ = ex_ % 2
        pb_ = k_ % 2

        def trE(e):
            ins = None
            for r_ in range(4):
                ins = e.transpose(bankb(pb_)[:, r_ * 128:(r_ + 1) * 128], xe[b_][:, r_, k_ * 128:(k_ + 1) * 128], ident)
            return ins
        T(trE, r=[R_xe[b_], CONST], w=[PB[pb_]])
        if k_ % 2 == 0:
            A(lambda e: e.activation(out=hTe2[b_][:, k_, :], in_=bankb(pb_)[:, 0:512], func=AF.Copy),
              r=[PB[pb_]], w=[R_hTe2[b_][k_]])
        else:
            V(lambda e: e.tensor_copy(hTe2[b_][:, k_, :], bankb(pb_)[:, 0:512]), r=[PB[pb_]], w=[R_hTe2[b_][k_]])

    def gateup(ex_, m_):
        b_ = ex_ % 2
        bg = 2 + (m_ % 2) * 2
        bu = bg + 1

        def mmG(e):
            ins = None
            for k in range(16):
                ins = e.matmul(bank(bg), wgb[b_][:, k, m_ * 128:(m_ + 1) * 128], hTe2[b_][:, k, :], start=(k == 0), stop=(k == 15))
            return ins

        def mmU(e):
            ins = None
            for k in range(16):
                ins = e.matmul(bank(bu), wub[b_][:, k, m_ * 128:(m_ + 1) * 128], hTe2[b_][:, k, :], start=(k == 0), stop=(k == 15))
            return ins
        T(mmG, r=R_hTe2[b_] + [R_wg[b_]], w=[PB[bg]])
        T(mmU, r=R_hTe2[b_] + [R_wu[b_]], w=[PB[bu]])
        A(lambda e: e.activation(out=sg[m_ % 2], in_=bank(bg), func=AF.Silu), r=[PB[bg]], w=[R_sg[m_ % 2]])
        V(lambda e: e.tensor_tensor(aT[:, m_, :], sg[m_ % 2], bank(bu), ALU.mult),
          r=[R_sg[m_ % 2], PB[bu]], w=[R_aT[m_]])

    def down(ex_, r_, yb_):
        b_ = ex_ % 2
        for nsl in range(4):
            bk = 6 + (nsl % 2)

            def mmD(e, nsl=nsl, bk=bk):
                ins = None
                for k in range(4):
                    ins = e.matmul(bank(bk), aT[:, k, r_ * 128:(r_ + 1) * 128], wdb[b_][:, k, nsl * 512:(nsl + 1) * 512],
                                   start=(k == 0), stop=(k == 3))
                return ins
            T(mmD, r=R_aT + [R_wd[b_]], w=[PB[bk]])
            if nsl % 2 == 0:
                A(lambda e, nsl=nsl, bk=bk: e.activation(out=ye[yb_][:, nsl * 512:(nsl + 1) * 512], in_=bank(bk), func=AF.Copy),
                  r=[PB[bk]], w=[R_ye[yb_]])
            else:
                V(lambda e, nsl=nsl, bk=bk: e.tensor_copy(ye[yb_][:, nsl * 512:(nsl + 1) * 512], bank(bk)),
                  r=[PB[bk]], w=[R_ye[yb_]])
        row0 = ex_ * CAP + r_ * 128
        DS(lambda e: e.dma_start(out=YS[row0:row0 + 128, :], in_=ye[yb_]), r=[R_ye[yb_]])

    NEX = NE
    load_xe(0)
    load_xe(1)
    load_we(0)
    for k in range(16):
        tr_chunk(0, k)
    yctr = 0
    for ex in range(NEX):
        if ex + 1 < NEX:
            load_we(ex + 1)
        if ex + 2 < NEX:
            load_xe(ex + 2)
        nxt = ex + 1 < NEX
        for m in range(4):
            gateup(ex, m)
            if nxt:
                tr_chunk(ex + 1, 2 * m)
                tr_chunk(ex + 1, 2 * m + 1)
        for r in range(4):
            down(ex, r, yctr % 2)
            yctr += 1
            if nxt:
                tr_chunk(ex + 1, 8 + 2 * r)
                tr_chunk(ex + 1, 8 + 2 * r + 1)
    P.barrier()
    if stop_after == "p2":
        P.emit()
        return nc

    ar.off = const_end
    G2 = ar.alloc([D], F32)
    FG = ar.alloc([D], F32)
    x1b = [ar.alloc([D], F32) for _ in range(2)]
    y1b = [ar.alloc([D], BF16) for _ in range(2)]
    y2b = [ar.alloc([D], BF16) for _ in range(2)]
    yab = [ar.alloc([D], F32) for _ in range(2)]
    R_ya3 = [Res("ya3_0"), Res("ya3_1")]
    junk = ar.alloc([D], BF16)
    ssq3 = [ar.alloc([1], F32) for _ in range(2)]
    R_G2, R_FG = Res("G2"), Res("FG")
    R_x1 = [Res("x1_0"), Res("x1_1")]
    R_y1 = [Res("y1_0"), Res("y1_1")]
    R_y2 = [Res("y2_0"), Res("y2_1")]
    R_s3 = [Res("s3_0"), Res("s3_1")]
    R_junk = Res("junk")
    DS(lambda e: e.dma_start(out=FG, in_=final_g[0:1, :].to_broadcast([128, D])), w=[R_FG])

    def load3(tt):
        b = tt % 2
        DS(lambda e: e.dma_start(out=x1b[b], in_=X1[tt * 128:(tt + 1) * 128, :]), w=[R_x1[b]])
        DG(lambda e: e.indirect_dma_start(out=y1b[b][:, :], out_offset=None, in_=YS[:, :],
                                          in_offset=bass.IndirectOffsetOnAxis(ap=DEST1[:, tt:tt + 1], axis=0),
                                          ), r=[RT], w=[R_y1[b]])
        DG(lambda e: e.indirect_dma_start(out=y2b[b][:, :], out_offset=None, in_=YS[:, :],
                                          in_offset=bass.IndirectOffsetOnAxis(ap=DEST2[:, tt:tt + 1], axis=0),
                                          ), r=[RT], w=[R_y2[b]])
    def make_tile3(tt):
        b = tt % 2
        sq_ = tt // 16

        def f0():
            if tt % 16 == 0:
                DS(lambda e: e.dma_start(out=G2, in_=MOD[sq_:sq_ + 1, 10240:12288].to_broadcast([128, D])), w=[R_G2])
            V(lambda e: e.tensor_scalar(yab[b], y1b[b], WT1[:, tt:tt + 1], None, ALU.mult), r=[R_y1[b], RT], w=[R_ya3[b]])
            V(lambda e: e.scalar_tensor_tensor(yab[b], y2b[b], WT2[:, tt:tt + 1], yab[b], ALU.mult, ALU.add),
              r=[R_ya3[b], R_y2[b], RT], w=[R_ya3[b]])
            V(lambda e: e.tensor_tensor(yab[b], yab[b], G2, ALU.mult), r=[R_ya3[b], R_G2], w=[R_ya3[b]])
            G(lambda e: e.tensor_tensor(x1b[b], x1b[b], yab[b], ALU.add), r=[R_ya3[b], R_x1[b]], w=[R_x1[b]])
            V(lambda e: e.memset(ssq3[b], 0.0), w=[R_s3[b]])
            A(lambda e: e.activation(out=junk, in_=x1b[b], func=AF.Square, accum_out=ssq3[b]), r=[R_x1[b], R_s3[b]], w=[R_junk, R_s3[b]])
            A(lambda e: e.activation(out=ssq3[b], in_=ssq3[b], func=AF.Sqrt, bias=EPS, scale=1.0 / D), r=[R_s3[b]], w=[R_s3[b]])

        def f1():
            V(lambda e: e.reciprocal(ssq3[b], ssq3[b]), r=[R_s3[b]], w=[R_s3[b]])
            V(lambda e: e.scalar_tensor_tensor(yab[b], x1b[b], ssq3[b], FG, ALU.mult, ALU.mult),
              r=[R_x1[b], R_s3[b], R_FG, R_ya3[b]], w=[R_ya3[b]])
            DS(lambda e: e.dma_start(out=out[tt * 128:(tt + 1) * 128, :], in_=yab[b]), r=[R_ya3[b]])
            if tt + 2 < 32:
                load3(tt + 2)
        return [f0, f1]
    load3(0)
    load3(1)
    emit_skewed([make_tile3(tt) for tt in range(32)], order=(0, 1))
    P.barrier()
    P.emit()
    return nc


def make_in_maps(inputs):
    f = lambda a: np.ascontiguousarray(np.asarray(a, dtype=np.float32))
    x = f(inputs["x"])
    c = f(inputs["c"])
    conv3 = f(inputs["conv3_w"])[0]
    conv4 = f(inputs["conv4_w"])[0]

    def pc(v):
        return np.ascontiguousarray(v.reshape(8, 128).T)
    smallp = np.zeros((128, 56), np.float32)
    for k in range(3):
        smallp[:, k:24:3] = pc(conv3[k])
    for k in range(4):
        smallp[:, 24 + k:56:4] = pc(conv4[k])
    smallp2 = np.concatenate([pc(f(inputs["conv4_b"])[0]), pc(f(inputs["lru_b_a"])[0]), pc(f(inputs["lru_b_x"])[0]),
                              pc(f(inputs["lru_lambda"])[0]), pc(f(inputs["head_norm_conv_g"])[0]),
                              pc(f(inputs["head_norm_lru_g"])[0])], axis=1)
    rwg = f(inputs["route_w_group"])[0]
    rwe = f(inputs["route_w_expert"])[0]
    wr = np.ascontiguousarray(np.concatenate([rwg, rwe.transpose(1, 0, 2).reshape(D, 64)], axis=1))
    br = np.ascontiguousarray(np.concatenate([f(inputs["route_b_group"])[0], f(inputs["route_b_expert"])[0].reshape(64)])[None, :])
    shared = {
        "ada_w": f(inputs["ada_w"])[0], "ada_b": f(inputs["ada_b"]),
        "norm1_g": f(inputs["norm1_g"]), "norm2_g": f(inputs["norm2_g"]),
        "final_g": f(inputs["final_norm_g"])[None, :],
        "w_in": f(inputs["w_in"])[0], "w_out": f(inputs["w_out"])[0],
        "smallp": smallp, "smallp2": np.ascontiguousarray(smallp2),
        "lru_w_a": f(inputs["lru_w_a"])[0], "lru_w_x": f(inputs["lru_w_x"])[0],
        "wr": wr, "br": br,
        "w_g": f(inputs["w_e_gate"])[0], "w_u": f(inputs["w_e_up"])[0], "w_d": f(inputs["w_e_down"])[0],
    }
    maps = []
    for i in range(NCORES):
        m = dict(shared)
        m["x"] = np.ascontiguousarray(x[2 * i:2 * i + 2].reshape(NTOK, D))
        cc = c[2 * i:2 * i + 2]
        m["cT"] = np.ascontiguousarray(cc.reshape(2, 16, 128).transpose(2, 1, 0).reshape(128, 32))
        maps.append(m)
    return maps


def kernel(**inputs):
    nc = build()
    maps = make_in_maps(inputs)
    res = run_bass_kernel_spmd(nc, maps, core_ids=list(range(NCORES)))
    outs = [np.asarray(r["out"]).reshape(2, S, D) for r in res.results]
    return np.concatenate(outs, axis=0).astype(np.float32)
```

```python
import numpy as np
from contextlib import ExitStack
import concourse.bass as bass
import concourse.mybir as mybir
from concourse.bass_utils import run_bass_kernel_spmd

F32 = mybir.dt.float32
BF16 = mybir.dt.bfloat16
I32 = mybir.dt.int32
AF = mybir.ActivationFunctionType
ALU = mybir.AluOpType
AX = mybir.AxisListType

D = 2048
S = 2048
NSEQ = 2
NTOK = NSEQ * S
DIN = 5120
NE = 64
DE = 512
CAP = 512
EPS = 1e-6
NCORES = 8
ARENA_BYTES = 211000
BIG = 1.0e4

ENGS = ("tensor", "vector", "scalar", "gpsimd", "sync")
NDMASEM = 16


class Res:
    __slots__ = ("name", "w", "r")

    def __init__(self, name):
        self.name = name
        self.w = None
        self.r = {}


class Prog:
    def __init__(self, nc, es):
        self.nc = nc
        self.streams = {e: [] for e in ENGS}
        self.cnt = {e: 0 for e in ENGS}
        self.seen = {e: {} for e in ENGS}
        self.esem = {e: es.enter_context(nc.semaphore("cs_" + e)) for e in ENGS if e != "sync"}
        self.dsem = {q: [es.enter_context(nc.semaphore("ds_%s_%d" % (q, i))) for i in range(NDMASEM)]
                     for q in ("sync", "gpsimd")}
        self.dn = {"sync": 0, "gpsimd": 0}

    def _sem(self, key):
        if key[0] == "eng":
            return self.esem[key[1]]
        return self.dsem[key[1]][key[2]]

    def op(self, eng, fn, reads=(), writes=(), dma=False):
        waits = {}
        seen = self.seen[eng]

        def need(tok):
            if tok is None:
                return
            k, v = tok
            if eng == "tensor" and k == ("eng", "tensor"):
                return
            if seen.get(k, 0) >= v:
                return
            if waits.get(k, 0) < v:
                waits[k] = v

        for r in reads:
            need(r.w)
        for w_ in writes:
            need(w_.w)
            for k, v in w_.r.items():
                need((k, v))
        if dma:
            j = self.dn[eng]
            self.dn[eng] += 1
            sidx = j % NDMASEM
            rnd = j // NDMASEM
            k = ("dma", eng, sidx)
            if rnd > 0:
                need((k, 16 * rnd))
            tok = (k, 16 * (rnd + 1))
            inc = (self._sem(k), 16)
        else:
            self.cnt[eng] += 1
            k = ("eng", eng)
            tok = (k, self.cnt[eng])
            inc = (self.esem[eng], 1)
        for k_, v in waits.items():
            seen[k_] = v
        self.streams[eng].append(([(self._sem(k_), v) for k_, v in waits.items()], fn, inc))
        for r in reads:
            if r.r.get(tok[0], 0) < tok[1]:
                r.r[tok[0]] = tok[1]
        for w_ in writes:
            w_.w = tok
            w_.r = {}
        return tok

    def barrier(self):
        toks = []
        for e in ENGS:
            if e != "sync" and self.cnt[e] > 0:
                toks.append((("eng", e), self.cnt[e]))
        for q in ("sync", "gpsimd"):
            n = self.dn[q]
            for sidx in range(min(n, NDMASEM)):
                rounds = (n - 1 - sidx) // NDMASEM + 1
                toks.append((("dma", q, sidx), 16 * rounds))
        for e in ENGS:
            waits = []
            for k, v in toks:
                if e == "tensor" and k == ("eng", "tensor"):
                    continue
                if self.seen[e].get(k, 0) >= v:
                    continue
                self.seen[e][k] = v
                waits.append((self._sem(k), v))
            if waits:
                self.streams[e].append((waits, None, None))

    def emit(self):
        with self.nc.Block() as blk:
            def mk(eng):
                def f(e):
                    for waits, fn, inc in self.streams[eng]:
                        for sem, v in waits:
                            e.wait_ge(sem, v)
                        if fn is not None:
                            ins = fn(e)
                            ins.then_inc(inc[0], inc[1])
                return f
            blk.sync(mk("sync"))
            blk.tensor(mk("tensor"))
            blk.vector(mk("vector"))
            blk.scalar(mk("scalar"))
            blk.gpsimd(mk("gpsimd"))


class Arena:
    def __init__(self, nc):
        self.t = nc.alloc_sbuf_tensor("arena", [128, ARENA_BYTES // 2], BF16)
        self.off = 0

    def alloc(self, shape, dt):
        n = int(np.prod(shape))
        nb = n * (2 if dt == BF16 else 4)
        nb = (nb + 31) // 32 * 32
        assert self.off + nb <= ARENA_BYTES, ("arena overflow", self.off, nb)
        a = self.t[:, self.off // 2:(self.off + nb) // 2]
        self.off += nb
        if dt != BF16:
            a = a.bitcast(dt)
        a = a[:, 0:n]
        if len(shape) == 2:
            a = a.rearrange("p (a b) -> p a b", a=shape[0])
        elif len(shape) == 3:
            a = a.rearrange("p (a b c) -> p a b c", a=shape[0], b=shape[1])
        return a


def build(stop_after="p3", debug=False):
    nc = bass.Bass("TRN2", target_bir_lowering=False)
    es = ExitStack()

    def din(name, shape, dt=F32):
        return nc.dram_tensor(name, shape, dt, kind="ExternalInput").ap()

    x = din("x", [NTOK, D])
    cT = din("cT", [128, 32])
    ada_w = din("ada_w", [D, 6 * D])
    ada_b = din("ada_b", [1, 6 * D])
    norm1_g = din("norm1_g", [1, D])
    norm2_g = din("norm2_g", [1, D])
    final_g = din("final_g", [1, D])
    w_in = din("w_in", [D, DIN])
    w_out = din("w_out", [D, D])
    NP = 56
    smallp = din("smallp", [128, NP])
    lru_w_a = din("lru_w_a", [8, 128, 128])
    lru_w_x = din("lru_w_x", [8, 128, 128])
    wr = din("wr", [D, 72])
    br = din("br", [1, 72])
    w_g = din("w_g", [NE, D, DE])
    w_u = din("w_u", [NE, D, DE])
    w_d = din("w_d", [NE, DE, D])
    out = nc.dram_tensor("out", [NTOK, D], F32, kind="ExternalOutput").ap()
    kind_s = "Internal"
    MOD = nc.dram_tensor("MOD", [2, 6 * D], F32, kind=kind_s).ap()
    X1 = nc.dram_tensor("X1", [NTOK, D], F32, kind=("ExternalOutput" if debug else "Internal")).ap()
    HS = nc.dram_tensor("HS", [NE * CAP + 128, D], BF16, kind=kind_s).ap()
    H2D = nc.dram_tensor("H2D", [NTOK, D], BF16, kind="Internal").ap()
    YS = nc.dram_tensor("YS", [NE * CAP, D], BF16, kind=kind_s).ap()
    if debug:
        DBG = nc.dram_tensor("DBG", [128, 256 + 1152], F32, kind="ExternalOutput").ap()
        DBGY = nc.dram_tensor("DBGY", [16 * 128, S], F32, kind="ExternalOutput").ap()

    P = Prog(nc, es)
    ar = Arena(nc)
    psum = es.enter_context(nc.psum_tensor("psum", [128, 4096], F32))
    psb = psum[:, :].bitcast(BF16)

    def bank(i):
        return psum[:, i * 512:(i + 1) * 512]

    def bankb(i):
        return psb[:, i * 1024:(i + 1) * 1024]

    PB = [Res("bank%d" % i) for i in range(8)]

    def V(fn, r=(), w=()):
        return P.op("vector", fn, r, w)

    def A(fn, r=(), w=()):
        return P.op("scalar", fn, r, w)

    def G(fn, r=(), w=()):
        return P.op("gpsimd", fn, r, w)

    def T(fn, r=(), w=()):
        return P.op("tensor", fn, r, w)

    def DS(fn, r=(), w=()):
        return P.op("sync", fn, r, w, dma=True)

    def DG(fn, r=(), w=()):
        return P.op("gpsimd", fn, r, w, dma=True)

    ident = ar.alloc([128], BF16)
    ltri = ar.alloc([128], BF16)
    onesb = ar.alloc([128], BF16)
    hm64 = ar.alloc([128], BF16)
    hm128 = ar.alloc([128], BF16)
    identf = ar.alloc([128], F32)
    iota_t = ar.alloc([128], F32)
    iota64 = ar.alloc([64], F32)
    sp = ar.alloc([NP], F32)
    klam = ar.alloc([8], F32)
    klam2 = ar.alloc([8], F32)
    ktmp = ar.alloc([8], F32)
    lwa = ar.alloc([8, 128], BF16)
    lwx = ar.alloc([8, 128], BF16)
    tot = ar.alloc([64], F32)
    DEST1 = ar.alloc([32], I32)
    DEST2 = ar.alloc([32], I32)
    WT1 = ar.alloc([32], F32)
    WT2 = ar.alloc([32], F32)
    CONST = Res("const")
    RT = Res("routing_persist")
    TOT = Res("tot")
    C3 = 0
    C4 = 24
    C4B = 56 - 0
    NP2 = 48
    smallp2 = din("smallp2", [128, NP2])
    sp2 = ar.alloc([NP2], F32)

    G(lambda e: e.iota(iota_t, pattern=[[1, 128]], base=0, channel_multiplier=-1,
                       allow_small_or_imprecise_dtypes=True), w=[CONST])
    V(lambda e: e.tensor_single_scalar(ident, iota_t, 0.0, ALU.is_equal), r=[CONST], w=[CONST])
    V(lambda e: e.tensor_single_scalar(identf, iota_t, 0.0, ALU.is_equal), r=[CONST], w=[CONST])
    V(lambda e: e.tensor_single_scalar(ltri, iota_t, 0.0, ALU.is_gt), r=[CONST], w=[CONST])
    V(lambda e: e.memset(onesb, 1.0), w=[CONST])
    V(lambda e: e.memset(hm128, 1.0 / 128), w=[CONST])
    V(lambda e: e.memset(hm64, 0.0), w=[CONST])
    V(lambda e: e.memset(hm64[0:64, 0:64], 1.0 / 64), r=[CONST], w=[CONST])
    V(lambda e: e.memset(hm64[64:128, 64:128], 1.0 / 64), r=[CONST], w=[CONST])
    V(lambda e: e.memset(tot, 0.0), w=[TOT])
    G(lambda e: e.iota(iota64, pattern=[[1, 64]], base=0, channel_multiplier=0,
                       allow_small_or_imprecise_dtypes=True), r=[CONST], w=[CONST])
    DS(lambda e: e.dma_start(out=sp, in_=smallp[:, :]), w=[CONST])
    DS(lambda e: e.dma_start(out=sp2, in_=smallp2[:, :]), r=[CONST], w=[CONST])
    DG(lambda e: e.dma_start(out=lwa, in_=lru_w_a.rearrange("h i j -> i h j")), r=[CONST], w=[CONST])
    DG(lambda e: e.dma_start(out=lwx, in_=lru_w_x.rearrange("h i j -> i h j")), r=[CONST], w=[CONST])
    A(lambda e: e.activation(out=ktmp, in_=sp2[:, 24:32], func=AF.Exp, scale=-1.0), r=[CONST], w=[CONST])
    A(lambda e: e.activation(out=ktmp, in_=ktmp, func=AF.Ln, bias=1.0, scale=1.0), r=[CONST], w=[CONST])
    V(lambda e: e.tensor_scalar(klam, ktmp, -8.0, None, ALU.mult), r=[CONST], w=[CONST])
    V(lambda e: e.tensor_scalar(klam2, ktmp, -16.0, None, ALU.mult), r=[CONST], w=[CONST])
    P.barrier()
    CONST.w = None
    CONST.r = {}
    const_end = ar.off

    cts = ar.alloc([32], F32)
    siluT = ar.alloc([16, 2], BF16)
    adaw = [ar.alloc([16, 512], BF16) for _ in range(3)]
    abt = [ar.alloc([512], F32) for _ in range(2)]
    modrow = [ar.alloc([512], F32) for _ in range(2)]
    R_adaw = [Res("adaw%d" % i) for i in range(3)]
    R_abt = [Res("abt%d" % i) for i in range(2)]
    R_mr = [Res("mr%d" % i) for i in range(2)]
    R_silu = Res("silu")
    DS(lambda e: e.dma_start(out=cts, in_=cT[:, :]), w=[R_silu])
    A(lambda e: e.activation(out=siluT.rearrange("p a b -> p (a b)"), in_=cts, func=AF.Silu), r=[R_silu], w=[R_silu])
    for j in range(24):
        sl = j % 3
        s2 = j % 2
        DG(lambda e, j=j, sl=sl: e.dma_start(
            out=adaw[sl], in_=ada_w[:, j * 512:(j + 1) * 512].rearrange("(k p) n -> p k n", p=128)),
            w=[R_adaw[sl]])
        DS(lambda e, j=j, s2=s2: e.dma_start(
            out=abt[s2][0:2, :], in_=ada_b[0:1, j * 512:(j + 1) * 512].to_broadcast([2, 512])), w=[R_abt[s2]])

        def mm0(e, sl=sl, s2=s2):
            ins = None
            for k in range(16):
                ins = e.matmul(bank(s2)[0:2, :], siluT[:, k, :], adaw[sl][:, k, :], start=(k == 0), stop=(k == 15))
            return ins
        T(mm0, r=[R_silu, R_adaw[sl]], w=[PB[s2]])
        V(lambda e, s2=s2: e.tensor_tensor(modrow[s2][0:2, :], bank(s2)[0:2, :], abt[s2][0:2, :], ALU.add),
          r=[PB[s2], R_abt[s2]], w=[R_mr[s2]])
        DS(lambda e, j=j, s2=s2: e.dma_start(out=MOD[0:2, j * 512:(j + 1) * 512], in_=modrow[s2][0:2, :]),
           r=[R_mr[s2]])
    P.barrier()
    if stop_after == "p0":
        P.emit()
        return nc

    ar.off = const_end
    R1 = ar.alloc([16, S], BF16)
    ynT = ar.alloc([16, S], BF16)
    stage_base = ar.off

    def emit_skewed(tiles, order=None):
        depth = max(len(t_) for t_ in tiles)
        for step in range(len(tiles) + depth - 1):
            for k in (order if order is not None else list(range(1, depth)) + [0]):
                j = step - k
                if 0 <= j < len(tiles) and k < len(tiles[j]):
                    tiles[j][k]()

    for s in range(NSEQ):
        ar.off = stage_base
        Abc = ar.alloc([D], F32)
        Bbc = ar.alloc([D], F32)
        xtb = [ar.alloc([D], F32) for _ in range(2)]
        t1 = ar.alloc([D], F32)
        hb = [ar.alloc([D], BF16) for _ in range(2)]
        ssq = [ar.alloc([1], F32) for _ in range(2)]
        srt = [ar.alloc([1], F32) for _ in range(2)]
        rstd = [ar.alloc([1], F32) for _ in range(2)]
        R_A, R_B = Res("Abc"), Res("Bbc")
        R_xt = [Res("xt0"), Res("xt1")]
        R_t1 = Res("t1")
        R_hb = [Res("hb0"), Res("hb1")]
        R_st = [Res("st0"), Res("st1")]
        R_hT = [Res("hT%d" % t) for t in range(16)]
        hT = R1
        DS(lambda e, s=s: e.dma_start(out=Abc, in_=MOD[s:s + 1, 2048:4096].to_broadcast([128, D])), w=[R_A])
        DS(lambda e: e.dma_start(out=t1, in_=norm1_g[0:1, :].to_broadcast([128, D])), w=[R_t1])
        DS(lambda e, s=s: e.dma_start(out=Bbc, in_=MOD[s:s + 1, 0:2048].to_broadcast([128, D])), w=[R_B])
        V(lambda e: e.scalar_tensor_tensor(Abc, Abc, 1.0, t1, ALU.add, ALU.mult), r=[R_t1, R_A], w=[R_A])

        def load_x(t, s=s):
            b = t % 2
            DS(lambda e: e.dma_start(out=xtb[b], in_=x[s * S + t * 128:s * S + (t + 1) * 128, :]), w=[R_xt[b]])
        t1b = [t1, ar.alloc([D], F32)]
        R_t1b = [R_t1, Res("t1b")]

        def make_tileA(t):
            b = t % 2

            def a0_():
                if t + 1 < 16:
                    load_x(t + 1)
                V(lambda e: e.memset(ssq[b], 0.0), w=[R_st[b]])
                A(lambda e: e.activation(out=hb[b], in_=xtb[b], func=AF.Square, accum_out=ssq[b]),
                  r=[R_xt[b]], w=[R_hb[b], R_st[b]])
                A(lambda e: e.activation(out=srt[b], in_=ssq[b], func=AF.Sqrt, bias=EPS, scale=1.0 / D),
                  r=[R_st[b]], w=[R_st[b]])
                V(lambda e: e.reciprocal(rstd[b], srt[b]), r=[R_st[b]], w=[R_st[b]])
                V(lambda e: e.scalar_tensor_tensor(t1b[b], xtb[b], rstd[b], Abc, ALU.mult, ALU.mult),
                  r=[R_xt[b], R_st[b], R_A], w=[R_t1b[b]])
                G(lambda e: e.tensor_tensor(hb[b], t1b[b], Bbc, ALU.add), r=[R_t1b[b], R_B], w=[R_hb[b]])

            def a1_():
                def trA(e):
                    ins = None
                    for k in range(16):
                        bk = 2 * b + k // 8
                        ins = e.transpose(bankb(bk)[:, (k % 8) * 128:(k % 8 + 1) * 128], hb[b][:, k * 128:(k + 1) * 128], ident)
                    return ins
                T(trA, r=[R_hb[b], CONST], w=[PB[2 * b], PB[2 * b + 1]])
                A(lambda e: e.activation(out=hT[:, 0:8, t * 128:(t + 1) * 128],
                                         in_=bankb(2 * b).rearrange("p (a b) -> p a b", a=8), func=AF.Copy),
                  r=[PB[2 * b]], w=[R_hT[t]])
                V(lambda e: e.tensor_copy(hT[:, 8:16, t * 128:(t + 1) * 128],
                                          bankb(2 * b + 1).rearrange("p (a b) -> p a b", a=8)),
                  r=[PB[2 * b + 1]], w=[R_hT[t]])
            return [a0_, a1_]
        load_x(0)
        emit_skewed([make_tileA(t) for t in range(16)], order=(0, 1))
        P.barrier()

        ar.off = stage_base
        NWS = 6
        win = [ar.alloc([16, 128], BF16) for _ in range(NWS)]
        R_win = [Res("win%d" % i) for i in range(NWS)]
        cxb = ar.alloc([3 + S], F32)
        xrb = cxb
        R_cx = [Res("cx%d" % n) for n in range(4)]
        R_xr = R_cx

        class TS:
            pass
        tsets = []
        for q in range(2):
            t_ = TS()
            for nm, dt_ in (("cs", F32), ("acc", F32), ("ya", F32), ("sqb", BF16), ("srtB", F32), ("xbb", BF16),
                            ("ii", F32), ("aa", F32), ("ml", F32), ("h", F32)):
                setattr(t_, nm, ar.alloc([512], dt_))
                setattr(t_, "R_" + nm, Res("%s%d" % (nm, q)))
            t_.rr, t_.R_rr = t_.srtB, t_.R_srtB
            t_.uu, t_.R_uu = t_.ml, t_.R_ml
            tsets.append(t_)
        R_yn = [Res("yn%d" % c) for c in range(16)]
        V(lambda e: e.memset(cxb[:, 0:3], 0.0), w=[R_cx[0]])
        def load_w(c):
            if c < 8:
                cols = [c * 128, 1024 + c * 128, 2048 + c * 128]
            else:
                cols = [3072 + (c - 8) * 128, 4096 + (c - 8) * 128]
            slots = []
            for col in cols:
                sl = wstate[0] % NWS
                wstate[0] += 1
                DG(lambda e, sl=sl, col=col: e.dma_start(
                    out=win[sl], in_=w_in[:, col:col + 128].rearrange("(k p) n -> p k n", p=128)), w=[R_win[sl]])
                slots.append(sl)
            wslots[c] = slots

        def make_tile(c, n, par):
            st = par * 3
            ts = tsets[par]
            tp = tsets[1 - par]
            tk = slice(n * 512, (n + 1) * 512)
            c0 = 3 + n * 512

            def mm_main():
                if n == 1 and c + 1 < 16:
                    load_w(c + 1)
                for mi, sl in enumerate(wslots[c]):
                    def mmB(e, sl=sl, bk=st + mi):
                        ins = None
                        for k in range(16):
                            ins = e.matmul(bank(bk), win[sl][:, k, :], hT[:, k, tk], start=(k == 0), stop=(k == 15))
                        return ins
                    T(mmB, r=[R_win[sl]], w=[PB[st + mi]])
            if c < 8:
                pBk, pCk, pXk = st, st + 1, st + 2
                aux = 6 + (n % 2)
                rcx = [R_cx[n]] + ([R_cx[n - 1]] if n > 0 else [])

                def q0():
                    mm_main()
                    A(lambda e: e.activation(out=ts.cs, in_=bank(pCk), func=AF.Copy), r=[PB[pCk]], w=[ts.R_cs])
                    V(lambda e: e.tensor_tensor(cxb[:, c0:c0 + 512], ts.cs, bank(pXk), ALU.mult),
                      r=[ts.R_cs, PB[pXk]], w=[R_cx[n]])
                    V(lambda e: e.tensor_scalar(ts.acc, cxb[:, c0:c0 + 512], sp[:, c * 3 + 2:c * 3 + 3], None, ALU.mult),
                      r=rcx, w=[ts.R_acc])
                    for dk in (1, 2):
                        V(lambda e, dk=dk: e.scalar_tensor_tensor(
                            ts.acc, cxb[:, c0 - dk:c0 - dk + 512], sp[:, c * 3 + 2 - dk:c * 3 + 3 - dk], ts.acc, ALU.mult, ALU.add),
                          r=rcx + [ts.R_acc], w=[ts.R_acc])
                    V(lambda e: e.tensor_tensor(ts.ya, ts.acc, bank(pBk), ALU.mult), r=[ts.R_acc, PB[pBk]], w=[ts.R_ya])
                    A(lambda e: e.activation(out=ts.sqb, in_=ts.ya, func=AF.Square), r=[ts.R_ya], w=[ts.R_sqb])

                def q1():
                    T(lambda e: e.matmul(bank(aux), hm64, ts.sqb, start=True, stop=True), r=[ts.R_sqb], w=[PB[aux]])
                    A(lambda e: e.activation(out=ts.srtB, in_=bank(aux), func=AF.Sqrt, bias=EPS, scale=1.0),
                      r=[PB[aux]], w=[ts.R_srtB])
                    V(lambda e: e.reciprocal(ts.srtB, ts.srtB), r=[ts.R_srtB], w=[ts.R_srtB])
                    V(lambda e: e.scalar_tensor_tensor(ynT[:, c, tk], ts.ya, sp2[:, 32 + c:33 + c], ts.srtB, ALU.mult, ALU.mult),
                      r=[ts.R_ya, ts.R_srtB], w=[R_yn[c]])
                return [q0, q1]
            h8 = c - 8
            pGk, pXk, pSk = st, st + 1, st + 2
            rxr = [R_xr[n]] + ([R_xr[n - 1]] if n > 0 else [])

            def p0():
                mm_main()
                A(lambda e: e.activation(out=ts.cs, in_=bank(pGk), func=AF.Gelu_apprx_tanh), r=[PB[pGk]], w=[ts.R_cs])
                A(lambda e: e.activation(out=xrb[:, c0:c0 + 512], in_=bank(pXk), func=AF.Copy), r=[PB[pXk]], w=[R_xr[n]])
                V(lambda e: e.tensor_scalar(ts.acc, xrb[:, c0:c0 + 512], sp[:, 24 + h8 * 4 + 3:24 + h8 * 4 + 4],
                                            sp2[:, h8:h8 + 1], ALU.mult, ALU.add), r=rxr, w=[ts.R_acc])
                for dk in (1, 2, 3):
                    V(lambda e, dk=dk: e.scalar_tensor_tensor(
                        ts.acc, xrb[:, c0 - dk:c0 - dk + 512], sp[:, 24 + h8 * 4 + 3 - dk:24 + h8 * 4 + 4 - dk], ts.acc, ALU.mult, ALU.add),
                      r=rxr + [ts.R_acc], w=[ts.R_acc])
                G(lambda e: e.tensor_copy(ts.xbb, ts.acc), r=[ts.R_acc], w=[ts.R_xbb])

            def p1():
                T(lambda e: e.matmul(bank(6), lwa[:, h8, :], ts.xbb, start=True, stop=True), r=[ts.R_xbb, CONST], w=[PB[6]])
                T(lambda e: e.matmul(bank(7), lwx[:, h8, :], ts.xbb, start=True, stop=True), r=[ts.R_xbb, CONST], w=[PB[7]])
                A(lambda e: e.activation(out=ts.rr, in_=bank(6), func=AF.Sigmoid, bias=sp2[:, 8 + h8:9 + h8], scale=1.0),
                  r=[PB[6]], w=[ts.R_rr])
                A(lambda e: e.activation(out=ts.ii, in_=bank(7), func=AF.Sigmoid, bias=sp2[:, 16 + h8:17 + h8], scale=1.0),
                  r=[PB[7]], w=[ts.R_ii])
                A(lambda e: e.activation(out=ts.aa, in_=ts.rr, func=AF.Exp, scale=klam[:, h8:h8 + 1]), r=[ts.R_rr], w=[ts.R_aa])
                A(lambda e: e.activation(out=ts.ml, in_=ts.rr, func=AF.Exp, scale=klam2[:, h8:h8 + 1]), r=[ts.R_rr], w=[ts.R_ml])
                A(lambda e: e.activation(out=ts.ml, in_=ts.ml, func=AF.Sqrt, bias=1.0, scale=-1.0), r=[ts.R_ml], w=[ts.R_ml])
                V(lambda e: e.tensor_tensor(ts.uu, ts.ml, ts.ii, ALU.mult), r=[ts.R_ml, ts.R_ii], w=[ts.R_uu])
                V(lambda e: e.tensor_tensor(ts.uu, ts.uu, ts.acc, ALU.mult), r=[ts.R_uu, ts.R_acc], w=[ts.R_uu])
                if n == 0:
                    V(lambda e: e.tensor_tensor_scan(ts.h, ts.aa, ts.uu, 0.0, ALU.mult, ALU.add),
                      r=[ts.R_aa, ts.R_uu], w=[ts.R_h])
                else:
                    V(lambda e: e.tensor_tensor_scan(ts.h, ts.aa, ts.uu, tp.h[:, 511:512], ALU.mult, ALU.add),
                      r=[ts.R_aa, ts.R_uu, tp.R_h], w=[ts.R_h])
                V(lambda e: e.tensor_tensor(ts.ya, ts.h, ts.cs, ALU.mult), r=[ts.R_h, ts.R_cs], w=[ts.R_ya])
                A(lambda e: e.activation(out=ts.sqb, in_=ts.ya, func=AF.Square), r=[ts.R_ya], w=[ts.R_sqb])

            def p2():
                T(lambda e: e.matmul(bank(pSk), hm128, ts.sqb, start=True, stop=True), r=[ts.R_sqb], w=[PB[pSk]])
                A(lambda e: e.activation(out=ts.srtB, in_=bank(pSk), func=AF.Sqrt, bias=EPS, scale=1.0),
                  r=[PB[pSk]], w=[ts.R_srtB])
                V(lambda e: e.reciprocal(ts.srtB, ts.srtB), r=[ts.R_srtB], w=[ts.R_srtB])
                V(lambda e: e.scalar_tensor_tensor(ynT[:, c, tk], ts.ya, sp2[:, 40 + h8:41 + h8], ts.srtB, ALU.mult, ALU.mult),
                  r=[ts.R_ya, ts.R_srtB], w=[R_yn[c]])
            return [p0, p1, p2]

        wstate = [0]
        wslots = {}
        load_w(0)
        tilesB = []
        ntc = 0
        for c in range(16):
            for n in range(4):
                tilesB.append(make_tile(c, n, ntc % 2))
                ntc += 1
        emit_skewed(tilesB, order=(0, 1, 2))
        P.barrier()
        if debug and s == 0:
            for c in range(16):
                for n in range(4):
                    V(lambda e, c=c, n=n: e.tensor_copy(tsets[0].acc, ynT[:, c, n * 512:(n + 1) * 512]), w=[tsets[0].R_acc])
                    DS(lambda e, c=c, n=n: e.dma_start(out=DBGY[c * 128:(c + 1) * 128, n * 512:(n + 1) * 512], in_=tsets[0].acc), r=[tsets[0].R_acc])
            P.barrier()

        ar.off = stage_base
        wo = R1
        Abc = ar.alloc([D], F32)
        Bbc = ar.alloc([D], F32)
        lgall = ar.alloc([16, 72], F32)
        routing_base = ar.off
        G1 = ar.alloc([D], F32)
        xtC = [ar.alloc([D], F32) for _ in range(2)]
        h2f = ar.alloc([D], F32)
        h2b = ar.alloc([D], BF16)
        h2T = ar.alloc([16, 128], F32)
        wrt = ar.alloc([16, 72], F32)
        brbc = ar.alloc([72], F32)
        R_wr = Res("wr")
        DS(lambda e: e.dma_start(out=wrt, in_=wr.rearrange("(k p) n -> p k n", p=128)), w=[R_wr])
        DS(lambda e: e.dma_start(out=brbc, in_=br[0:1, :].to_broadcast([128, 72])), r=[R_wr], w=[R_wr])
        R_A, R_B, R_G1 = Res("A2"), Res("B2"), Res("G1")
        R_xtC = [Res("xtC0"), Res("xtC1")]
        R_h2f, R_h2b, R_h2T = Res("h2f"), Res("h2b"), Res("h2T")
        R_pmg2 = [Res("pmg0")] * 2
        R_wo = [Res("wo%d" % i) for i in range(4)]
        R_sm = Res("small")
        R_n2 = Res("n2small")

        def sm(n, dt=F32):
            return ar.alloc([n], dt)
        ssq2, srt2, rstd2 = sm(1), sm(1), sm(1)
        R_lg = Res('lgall')

        for q in range(4):
            DG(lambda e, q=q: e.dma_start(out=wo[:, q * 4:(q + 1) * 4, :],
                                          in_=w_out[q * 512:(q + 1) * 512, :].rearrange("(k p) n -> p k n", p=128)),
               w=[R_wo[q]])
        DS(lambda e, s=s: e.dma_start(out=Abc, in_=MOD[s:s + 1, 8192:10240].to_broadcast([128, D])), w=[R_A])
        DS(lambda e: e.dma_start(out=h2f, in_=norm2_g[0:1, :].to_broadcast([128, D])), w=[R_h2f])
        DS(lambda e, s=s: e.dma_start(out=Bbc, in_=MOD[s:s + 1, 6144:8192].to_broadcast([128, D])), w=[R_B])
        DS(lambda e, s=s: e.dma_start(out=G1, in_=MOD[s:s + 1, 4096:6144].to_broadcast([128, D])), w=[R_G1])
        V(lambda e: e.scalar_tensor_tensor(Abc, Abc, 1.0, h2f, ALU.add, ALU.mult), r=[R_h2f, R_A], w=[R_A])

        def make_tileC(t):
            col = s * 16 + t
            tok0 = s * S + t * 128
            xt = xtC[t % 2]
            R_xt = R_xtC[t % 2]

            def c0():
                DS(lambda e, tok0=tok0: e.dma_start(out=xt, in_=x[tok0:tok0 + 128, :]), w=[R_xt])
                for nsl in range(4):
                    def mmC(e, nsl=nsl, t=t):
                        ins = None
                        for k in range(16):
                            ins = e.matmul(bank(nsl), ynT[:, k, t * 128:(t + 1) * 128], wo[:, k, nsl * 512:(nsl + 1) * 512],
                                           start=(k == 0), stop=(k == 15))
                        return ins
                    T(mmC, r=R_wo, w=[PB[nsl]])
                    V(lambda e, nsl=nsl: e.tensor_tensor(bank(nsl), bank(nsl), G1[:, nsl * 512:(nsl + 1) * 512], ALU.mult),
                      r=[PB[nsl], R_G1], w=[PB[nsl]])
                    V(lambda e, nsl=nsl: e.tensor_tensor(xt[:, nsl * 512:(nsl + 1) * 512], xt[:, nsl * 512:(nsl + 1) * 512], bank(nsl), ALU.add),
                      r=[PB[nsl], R_xt], w=[R_xt])
                DS(lambda e, tok0=tok0: e.dma_start(out=X1[tok0:tok0 + 128, :], in_=xt), r=[R_xt])

            def c1():
                V(lambda e: e.memset(ssq2, 0.0), w=[R_n2])
                A(lambda e: e.activation(out=h2T.rearrange("p a b -> p (a b)"), in_=xt, func=AF.Square, accum_out=ssq2),
                  r=[R_xt, R_n2], w=[R_h2T, R_n2])
                A(lambda e: e.activation(out=srt2, in_=ssq2, func=AF.Sqrt, bias=EPS, scale=1.0 / D), r=[R_n2], w=[R_n2])
                V(lambda e: e.reciprocal(rstd2, srt2), r=[R_n2], w=[R_n2])
                V(lambda e: e.scalar_tensor_tensor(h2f, xt, rstd2, Abc, ALU.mult, ALU.mult), r=[R_xt, R_n2, R_A], w=[R_h2f])
                G(lambda e: e.tensor_tensor(h2f, h2f, Bbc, ALU.add), r=[R_h2f, R_B], w=[R_h2f])
                A(lambda e: e.activation(out=h2b, in_=h2f, func=AF.Copy), r=[R_h2f], w=[R_h2b])
                DS(lambda e: e.dma_start(out=H2D[tok0:tok0 + 128, :], in_=h2b), r=[R_h2b])
                for half in range(2):
                    def trC(e, half=half):
                        ins = None
                        for kk in range(8):
                            k = half * 8 + kk
                            ins = e.transpose(psum[:, (4 + half * 2) * 512 + kk * 128:(4 + half * 2) * 512 + (kk + 1) * 128],
                                              h2f[:, k * 128:(k + 1) * 128], identf)
                        return ins
                    T(trC, r=[R_h2f, CONST], w=[PB[4 + half * 2], PB[5 + half * 2]])
                    for q2 in range(2):
                        bkq = 4 + half * 2 + q2
                        srcq = bank(bkq).rearrange("p (a b) -> p a b", a=4)
                        k0 = half * 8 + q2 * 4
                        A(lambda e, srcq=srcq, k0=k0: e.activation(out=h2T[:, k0:k0 + 4, :], in_=srcq, func=AF.Copy),
                          r=[PB[bkq]], w=[R_h2T])

                def mmL(e):
                    ins = None
                    for k in range(16):
                        ins = e.matmul(bank(4)[:, 0:72], h2T[:, k, :], wrt[:, k, :], start=(k == 0), stop=(k == 15))
                    return ins
                T(mmL, r=[R_h2T, R_wr], w=[PB[4]])

                V(lambda e: e.tensor_tensor(lgall[:, t, :], bank(4)[:, 0:72], brbc, ALU.add), r=[PB[4], R_wr], w=[R_lg])

            return [c0, c1]

        tilesC = [make_tileC(t) for t in range(16)]
        emit_skewed(tilesC, order=(0, 1))
        P.barrier()
        ar.off = routing_base
        h2all = R1
        DS(lambda e, s=s: e.dma_start(out=h2all[:, 0:8, :], in_=H2D[s * S:s * S + 1024, :].rearrange("(t p) d -> p t d", p=128)), w=[R_h2b])
        DS(lambda e, s=s: e.dma_start(out=h2all[:, 8:16, :], in_=H2D[s * S + 1024:(s + 1) * S, :].rearrange("(t p) d -> p t d", p=128)), w=[R_xtC[0]])
        R_rt = Res("rt")
        rs = [R_rt]

        def a16(dt=F32):
            return ar.alloc([16], dt)

        def a168():
            return ar.alloc([16, 8], F32)

        def a1664(dt=F32):
            return ar.alloc([16, 64], dt)
        gmax, sumg, pg, v1, v2, dv, ev, den, w1s, w2s = [a16() for _ in range(10)]
        pp, ee_, ov, nov, d1, dsc = [a16() for _ in range(6)]
        dsci = [a16(I32), a16(I32)]
        tmpg, mg, eg, pen = a168(), a168(), a168(), a168()
        msk, oh1, oh2, tmpL, base = a1664(), a1664(), a1664(), a1664(), a1664()
        abf = a1664(BF16)
        lg_g = lgall[:, :, 0:8]
        lg_e4 = lgall[:, :, 8:72].rearrange("p t (g e) -> p t g e", g=8)
        msk4 = msk.rearrange("p t (g e) -> p t g e", g=8)
        fl = lambda a_: a_.rearrange("p a b -> p (a b)")
        bc8 = lambda a_: a_.unsqueeze(2).to_broadcast([128, 16, 8])
        bc64 = lambda a_: a_.unsqueeze(2).to_broadcast([128, 16, 64])
        V(lambda e: e.reduce_max(gmax, lg_g, AX.X), r=[R_lg], w=rs)
        V(lambda e: e.tensor_tensor(mg, lg_g, bc8(gmax), ALU.is_ge), r=rs, w=rs)
        V(lambda e: e.tensor_tensor(tmpg, lg_g, bc8(gmax), ALU.subtract), r=rs, w=rs)
        A(lambda e: e.activation(out=fl(eg), in_=fl(tmpg), func=AF.Exp), r=rs, w=rs)
        V(lambda e: e.reduce_sum(sumg, eg, AX.X), r=rs, w=rs)
        V(lambda e: e.reciprocal(pg, sumg), r=rs, w=rs)
        V(lambda e: e.tensor_scalar(fl(pen), fl(mg), BIG, -BIG, ALU.mult, ALU.add), r=rs, w=rs)
        for g in range(8):
            V(lambda e, g=g: e.tensor_tensor(msk[:, :, g * 8:(g + 1) * 8], lgall[:, :, 8 + g * 8:16 + g * 8],
                                             pen[:, :, g:g + 1].to_broadcast([128, 16, 8]), ALU.add), r=rs, w=rs)
        V(lambda e: e.reduce_max(v1, msk, AX.X), r=rs, w=rs)
        V(lambda e: e.tensor_tensor(oh1, msk, bc64(v1), ALU.is_equal), r=rs, w=rs)
        V(lambda e: e.scalar_tensor_tensor(fl(msk), fl(oh1), -BIG, fl(msk), ALU.mult, ALU.add), r=rs, w=rs)
        V(lambda e: e.reduce_max(v2, msk, AX.X), r=rs, w=rs)
        V(lambda e: e.tensor_tensor(oh2, msk, bc64(v2), ALU.is_equal), r=rs, w=rs)
        V(lambda e: e.tensor_tensor(dv, v2, v1, ALU.subtract), r=rs, w=rs)
        A(lambda e: e.activation(out=ev, in_=dv, func=AF.Exp), r=rs, w=rs)
        V(lambda e: e.tensor_scalar(den, ev, 1.0, None, ALU.add), r=rs, w=rs)
        V(lambda e: e.reciprocal(w1s, den), r=rs, w=rs)
        V(lambda e: e.tensor_tensor(w2s, ev, w1s, ALU.mult), r=rs, w=rs)
        V(lambda e: e.tensor_tensor(w1s, w1s, pg, ALU.mult), r=rs, w=rs)
        V(lambda e: e.tensor_tensor(w2s, w2s, pg, ALU.mult), r=rs, w=rs)
        V(lambda e: e.tensor_tensor(fl(abf), fl(oh1), fl(oh2), ALU.add), r=rs, w=rs)

        def mmPB(e):
            ins = None
            for half in range(2):
                e.matmul(bank(half), ltri, fl(abf)[:, half * 512:(half + 1) * 512], start=True, stop=True)
                ins = e.matmul(bank(2 + half), onesb, fl(abf)[:, half * 512:(half + 1) * 512], start=True, stop=True)
            return ins
        T(mmPB, r=[R_rt, CONST], w=[PB[0], PB[1], PB[2], PB[3]])
        V(lambda e: e.tensor_copy(base[:, 0, :], tot), r=[TOT, R_rt], w=rs)
        for j in range(1, 16):
            jj = j - 1
            V(lambda e, j=j, jj=jj: e.tensor_tensor(base[:, j, :], base[:, jj, :], bank(2 + jj // 8)[:, (jj % 8) * 64:(jj % 8 + 1) * 64], ALU.add),
              r=rs + [PB[2], PB[3]], w=rs)
        V(lambda e: e.tensor_tensor(tot, base[:, 15, :], bank(3)[:, 7 * 64:8 * 64], ALU.add), r=rs + [PB[3]], w=[TOT])
        posf = msk
        for half in range(2):
            V(lambda e, half=half: e.tensor_tensor(fl(posf)[:, half * 512:(half + 1) * 512], bank(half), fl(base)[:, half * 512:(half + 1) * 512], ALU.add),
              r=rs + [PB[half]], w=rs)
        c0_ = s * 16
        for (oh, wsl, DST, WTT, ji) in ((oh1, w1s, DEST1, WT1, 0), (oh2, w2s, DEST2, WT2, 1)):
            V(lambda e, oh=oh: e.tensor_tensor(fl(tmpL), fl(oh), fl(posf), ALU.mult), r=rs, w=rs)
            V(lambda e: e.reduce_sum(pp, tmpL, AX.X), r=rs, w=rs)
            V(lambda e, oh=oh: e.tensor_tensor(tmpL, oh, iota64.unsqueeze(1).to_broadcast([128, 16, 64]), ALU.mult), r=rs + [CONST], w=rs)
            V(lambda e: e.reduce_sum(ee_, tmpL, AX.X), r=rs, w=rs)
            V(lambda e: e.tensor_scalar(ov, pp, float(CAP), None, ALU.is_ge), r=rs, w=rs)
            V(lambda e: e.tensor_scalar(nov, ov, -1.0, 1.0, ALU.mult, ALU.add), r=rs, w=rs)
            V(lambda e: e.scalar_tensor_tensor(d1, ee_, float(CAP), pp, ALU.mult, ALU.add), r=rs, w=rs)
            V(lambda e: e.tensor_tensor(d1, d1, nov, ALU.mult), r=rs, w=rs)
            V(lambda e: e.scalar_tensor_tensor(dsc, ov, float(NE * CAP), d1, ALU.mult, ALU.add), r=rs, w=rs)
            V(lambda e, ji=ji: e.tensor_copy(dsci[ji], dsc), r=rs, w=rs)
            V(lambda e, DST=DST, c0_=c0_: e.tensor_copy(DST[:, c0_:c0_ + 16], d1), r=rs, w=[RT, R_rt])
            V(lambda e, WTT=WTT, wsl=wsl, c0_=c0_: e.tensor_tensor(WTT[:, c0_:c0_ + 16], wsl, nov, ALU.mult), r=rs, w=[RT, R_rt])
            for t in range(16):
                DG(lambda e, ji=ji, t=t: e.indirect_dma_start(
                    out=HS[:, :], out_offset=bass.IndirectOffsetOnAxis(ap=dsci[ji][:, t:t + 1], axis=0),
                    in_=h2all[:, t, :], in_offset=None),
                   r=[R_rt, R_h2b, R_xtC[0]])
        P.barrier()
    if debug:
        dbt = ar.alloc([128], F32)
        R_dbt = Res("dbt")
        V(lambda e: e.tensor_copy(dbt[:, 0:32], DEST1), r=[RT], w=[R_dbt])
        V(lambda e: e.tensor_copy(dbt[:, 32:64], DEST2), r=[RT, R_dbt], w=[R_dbt])
        V(lambda e: e.tensor_copy(dbt[:, 64:96], WT1), r=[RT, R_dbt], w=[R_dbt])
        V(lambda e: e.tensor_copy(dbt[:, 96:128], WT2), r=[RT, R_dbt], w=[R_dbt])
        DS(lambda e: e.dma_start(out=DBG[:, 0:128], in_=dbt), r=[R_dbt])
        dbt2 = ar.alloc([128], F32)
        V(lambda e: e.tensor_copy(dbt2[:, 0:16], gmax), w=[R_dbt])
        V(lambda e: e.tensor_copy(dbt2[:, 16:32], v1), r=[R_dbt], w=[R_dbt])
        V(lambda e: e.tensor_copy(dbt2[:, 32:40], lgall[:, 0, 0:8]), r=[R_dbt], w=[R_dbt])
        V(lambda e: e.tensor_copy(dbt2[:, 40:48], mg[:, 0, :]), r=[R_dbt], w=[R_dbt])
        V(lambda e: e.tensor_copy(dbt2[:, 48:56], pen[:, 0, :]), r=[R_dbt], w=[R_dbt])
        V(lambda e: e.tensor_copy(dbt2[:, 56:64], lgall[:, 5, 0:8]), r=[R_dbt], w=[R_dbt])
        V(lambda e: e.tensor_copy(dbt2[:, 64:128], lgall[:, 0, 8:72]), r=[R_dbt], w=[R_dbt])
        DS(lambda e: e.dma_start(out=DBG[:, 128:256], in_=dbt2), r=[R_dbt])
        DS(lambda e: e.dma_start(out=DBG[:, 256:256 + 1152], in_=lgall.rearrange("p a b -> p (a b)")), r=[R_dbt])
        P.barrier()
    if stop_after == "p1":
        P.emit()
        return nc

    ar.off = const_end
    xe = [ar.alloc([4, D], BF16) for _ in range(2)]
    wgb = [ar.alloc([16, DE], BF16) for _ in range(2)]
    wub = [ar.alloc([16, DE], BF16) for _ in range(2)]
    wdb = [ar.alloc([4, D], BF16) for _ in range(2)]
    hTe2 = [ar.alloc([16, CAP], BF16) for _ in range(2)]
    aT = ar.alloc([4, CAP], BF16)
    sg = [ar.alloc([512], F32) for _ in range(2)]
    ye = [ar.alloc([D], BF16) for _ in range(2)]
    R_xe = [Res("xe0"), Res("xe1")]
    R_wg = [Res("wg0"), Res("wg1")]
    R_wu = [Res("wu0"), Res("wu1")]
    R_wd = [Res("wd0"), Res("wd1")]
    R_hTe2 = [[Res("hTe%d_%d" % (q, k)) for k in range(16)] for q in range(2)]
    R_aT = [Res("aT%d" % m) for m in range(4)]
    R_sg = [Res("sg0"), Res("sg1")]
    R_ye = [Res("ye0"), Res("ye1")]

    def load_xe(ex_):
        b_ = ex_ % 2
        DS(lambda e: e.dma_start(out=xe[b_], in_=HS[ex_ * CAP:(ex_ + 1) * CAP, :].rearrange("(r p) d -> p r d", p=128)),
           w=[R_xe[b_]])

    def load_we(ex_):
        b_ = ex_ % 2
        DG(lambda e: e.dma_start(out=wgb[b_], in_=w_g[ex_].rearrange("(k p) n -> p k n", p=128)), w=[R_wg[b_]])
        DG(lambda e: e.dma_start(out=wub[b_], in_=w_u[ex_].rearrange("(k p) n -> p k n", p=128)), w=[R_wu[b_]])
        DG(lambda e: e.dma_start(out=wdb[b_], in_=w_d[ex_].rearrange("(k p) n -> p k n", p=128)), w=[R_wd[b_]])

    def tr_chunk(ex_, k_):
        b_ = ex_ % 2
        pb_ = k_ % 2

        def trE(e):
            ins = None
            for r_ in range(4):
                ins = e.transpose(bankb(pb_)[:, r_ * 128:(r_ + 1) * 128], xe[b_][:, r_, k_ * 128:(k_ + 1) * 128], ident)
            return ins
        T(trE, r=[R_xe[b_], CONST], w=[PB[pb_]])
        if k_ % 2 == 0:
            A(lambda e: e.activation(out=hTe2[b_][:, k_, :], in_=bankb(pb_)[:, 0:512], func=AF.Copy),
              r=[PB[pb_]], w=[R_hTe2[b_][k_]])
        else:
            V(lambda e: e.tensor_copy(hTe2[b_][:, k_, :], bankb(pb_)[:, 0:512]), r=[PB[pb_]], w=[R_hTe2[b_][k_]])

    def gateup(ex_, m_):
        b_ = ex_ % 2
        bg = 2 + (m_ % 2) * 2
        bu = bg + 1

        def mmG(e):
            ins = None
            for k in range(16):
                ins = e.matmul(bank(bg), wgb[b_][:, k, m_ * 128:(m_ + 1) * 128], hTe2[b_][:, k, :], start=(k == 0), stop=(k == 15))
            return ins

        def mmU(e):
            ins = None
            for k in range(16):
                ins = e.matmul(bank(bu), wub[b_][:, k, m_ * 128:(m_ + 1) * 128], hTe2[b_][:, k, :], start=(k == 0), stop=(k == 15))
            return ins
        T(mmG, r=R_hTe2[b_] + [R_wg[b_]], w=[PB[bg]])
        T(mmU, r=R_hTe2[b_] + [R_wu[b_]], w=[PB[bu]])
        A(lambda e: e.activation(out=sg[m_ % 2], in_=bank(bg), func=AF.Silu), r=[PB[bg]], w=[R_sg[m_ % 2]])
        V(lambda e: e.tensor_tensor(aT[:, m_, :], sg[m_ % 2], bank(bu), ALU.mult),
          r=[R_sg[m_ % 2], PB[bu]], w=[R_aT[m_]])

    def down(ex_, r_, yb_):
        b_ = ex_ % 2
        for nsl in range(4):
            bk = 6 + (nsl % 2)

            def mmD(e, nsl=nsl, bk=bk):
                ins = None
                for k in range(4):
                    ins = e.matmul(bank(bk), aT[:, k, r_ * 128:(r_ + 1) * 128], wdb[b_][:, k, nsl * 512:(nsl + 1) * 512],
                                   start=(k == 0), stop=(k == 3))
                return ins
            T(mmD, r=R_aT + [R_wd[b_]], w=[PB[bk]])
            if nsl % 2 == 0:
                A(lambda e, nsl=nsl, bk=bk: e.activation(out=ye[yb_][:, nsl * 512:(nsl + 1) * 512], in_=bank(bk), func=AF.Copy),
                  r=[PB[bk]], w=[R_ye[yb_]])
            else:
                V(lambda e, nsl=nsl, bk=bk: e.tensor_copy(ye[yb_][:, nsl * 512:(nsl + 1) * 512], bank(bk)),
                  r=[PB[bk]], w=[R_ye[yb_]])
        row0 = ex_ * CAP + r_ * 128
        DS(lambda e: e.dma_start(out=YS[row0:row0 + 128, :], in_=ye[yb_]), r=[R_ye[yb_]])

    NEX = NE
    load_xe(0)
    load_xe(1)
    load_we(0)
    for k in range(16):
        tr_chunk(0, k)
    yctr = 0
    for ex in range(NEX):
        if ex + 1 < NEX:
            load_we(ex + 1)
        if ex + 2 < NEX:
            load_xe(ex + 2)
        nxt = ex + 1 < NEX
        for m in range(4):
            gateup(ex, m)
            if nxt:
                tr_chunk(ex + 1, 2 * m)
                tr_chunk(ex + 1, 2 * m + 1)
        for r in range(4):
            down(ex, r, yctr % 2)
            yctr += 1
            if nxt:
                tr_chunk(ex + 1, 8 + 2 * r)
                tr_chunk(ex + 1, 8 + 2 * r + 1)
    P.barrier()
    if stop_after == "p2":
        P.emit()
        return nc

    ar.off = const_end
    G2 = ar.alloc([D], F32)
    FG = ar.alloc([D], F32)
    x1b = [ar.alloc([D], F32) for _ in range(3)]
    y1b = [ar.alloc([D], BF16) for _ in range(3)]
    y2b = [ar.alloc([D], BF16) for _ in range(3)]
    yab = [ar.alloc([D], F32) for _ in range(2)]
    R_ya3 = [Res("ya3_0"), Res("ya3_1")]
    junk = ar.alloc([D], BF16)
    ssq3 = [ar.alloc([1], F32) for _ in range(2)]
    R_G2, R_FG = Res("G2"), Res("FG")
    R_x1 = [Res("x1_%d" % i) for i in range(3)]
    R_y1 = [Res("y1_%d" % i) for i in range(3)]
    R_y2 = [Res("y2_%d" % i) for i in range(3)]
    R_s3 = [Res("s3_0"), Res("s3_1")]
    R_junk = Res("junk")
    DS(lambda e: e.dma_start(out=FG, in_=final_g[0:1, :].to_broadcast([128, D])), w=[R_FG])

    def load3(tt):
        b = tt % 3
        DS(lambda e: e.dma_start(out=x1b[b], in_=X1[tt * 128:(tt + 1) * 128, :]), w=[R_x1[b]])
        DG(lambda e: e.indirect_dma_start(out=y1b[b][:, :], out_offset=None, in_=YS[:, :],
                                          in_offset=bass.IndirectOffsetOnAxis(ap=DEST1[:, tt:tt + 1], axis=0),
                                          ), r=[RT], w=[R_y1[b]])
        DG(lambda e: e.indirect_dma_start(out=y2b[b][:, :], out_offset=None, in_=YS[:, :],
                                          in_offset=bass.IndirectOffsetOnAxis(ap=DEST2[:, tt:tt + 1], axis=0),
                                          ), r=[RT], w=[R_y2[b]])
    load3(0)
    load3(1)
    for tt in range(32):
        b = tt % 2
        b3 = tt % 3
        sq3 = tt // 16
        if tt % 16 == 0:
            DS(lambda e, sq3=sq3: e.dma_start(out=G2, in_=MOD[sq3:sq3 + 1, 10240:12288].to_broadcast([128, D])), w=[R_G2])
        if tt + 2 < 32:
            load3(tt + 2)
        V(lambda e, b=b, b3=b3, tt=tt: e.tensor_scalar(yab[b], y1b[b3], WT1[:, tt:tt + 1], None, ALU.mult), r=[R_y1[b3], RT], w=[R_ya3[b]])
        V(lambda e, b=b, b3=b3, tt=tt: e.scalar_tensor_tensor(yab[b], y2b[b3], WT2[:, tt:tt + 1], yab[b], ALU.mult, ALU.add),
          r=[R_ya3[b], R_y2[b3], RT], w=[R_ya3[b]])
        V(lambda e, b=b, b3=b3: e.tensor_tensor(yab[b], yab[b], G2, ALU.mult), r=[R_ya3[b], R_G2], w=[R_ya3[b]])
        G(lambda e, b=b, b3=b3: e.tensor_tensor(x1b[b3], x1b[b3], yab[b], ALU.add), r=[R_ya3[b], R_x1[b3]], w=[R_x1[b3]])
        V(lambda e, b=b, b3=b3: e.memset(ssq3[b], 0.0), w=[R_s3[b]])
        A(lambda e, b=b, b3=b3: e.activation(out=junk, in_=x1b[b3], func=AF.Square, accum_out=ssq3[b]), r=[R_x1[b3], R_s3[b]], w=[R_junk, R_s3[b]])
        A(lambda e, b=b, b3=b3: e.activation(out=ssq3[b], in_=ssq3[b], func=AF.Sqrt, bias=EPS, scale=1.0 / D), r=[R_s3[b]], w=[R_s3[b]])
        V(lambda e, b=b, b3=b3: e.reciprocal(ssq3[b], ssq3[b]), r=[R_s3[b]], w=[R_s3[b]])
        V(lambda e, b=b, b3=b3: e.scalar_tensor_tensor(yab[b], x1b[b3], ssq3[b], FG, ALU.mult, ALU.mult),
          r=[R_x1[b3], R_s3[b], R_FG, R_ya3[b]], w=[R_ya3[b]])
        DS(lambda e, b=b, b3=b3, tt=tt: e.dma_start(out=out[tt * 128:(tt + 1) * 128, :], in_=yab[b]), r=[R_ya3[b]])
    P.barrier()
    P.emit()
    return nc


def make_in_maps(inputs):
    f = lambda a: np.ascontiguousarray(np.asarray(a, dtype=np.float32))
    x = f(inputs["x"])
    c = f(inputs["c"])
    conv3 = f(inputs["conv3_w"])[0]
    conv4 = f(inputs["conv4_w"])[0]

    def pc(v):
        return np.ascontiguousarray(v.reshape(8, 128).T)
    smallp = np.zeros((128, 56), np.float32)
    for k in range(3):
        smallp[:, k:24:3] = pc(conv3[k])
    for k in range(4):
        smallp[:, 24 + k:56:4] = pc(conv4[k])
    smallp2 = np.concatenate([pc(f(inputs["conv4_b"])[0]), pc(f(inputs["lru_b_a"])[0]), pc(f(inputs["lru_b_x"])[0]),
                              pc(f(inputs["lru_lambda"])[0]), pc(f(inputs["head_norm_conv_g"])[0]),
                              pc(f(inputs["head_norm_lru_g"])[0])], axis=1)
    rwg = f(inputs["route_w_group"])[0]
    rwe = f(inputs["route_w_expert"])[0]
    wr = np.ascontiguousarray(np.concatenate([rwg, rwe.transpose(1, 0, 2).reshape(D, 64)], axis=1))
    br = np.ascontiguousarray(np.concatenate([f(inputs["route_b_group"])[0], f(inputs["route_b_expert"])[0].reshape(64)])[None, :])
    shared = {
        "ada_w": f(inputs["ada_w"])[0], "ada_b": f(inputs["ada_b"]),
        "norm1_g": f(inputs["norm1_g"]), "norm2_g": f(inputs["norm2_g"]),
        "final_g": f(inputs["final_norm_g"])[None, :],
        "w_in": f(inputs["w_in"])[0], "w_out": f(inputs["w_out"])[0],
        "smallp": smallp, "smallp2": np.ascontiguousarray(smallp2),
        "lru_w_a": f(inputs["lru_w_a"])[0], "lru_w_x": f(inputs["lru_w_x"])[0],
        "wr": wr, "br": br,
        "w_g": f(inputs["w_e_gate"])[0], "w_u": f(inputs["w_e_up"])[0], "w_d": f(inputs["w_e_down"])[0],
    }
    maps = []
    for i in range(NCORES):
        m = dict(shared)
        m["x"] = np.ascontiguousarray(x[2 * i:2 * i + 2].reshape(NTOK, D))
        cc = c[2 * i:2 * i + 2]
        m["cT"] = np.ascontiguousarray(cc.reshape(2, 16, 128).transpose(2, 1, 0).reshape(128, 32))
        maps.append(m)
    return maps


def kernel(**inputs):
    nc = build()
    maps = make_in_maps(inputs)
    res = run_bass_kernel_spmd(nc, maps, core_ids=list(range(NCORES)))
    outs = [np.asarray(r["out"]).reshape(2, S, D) for r in res.results]
    return np.concatenate(outs, axis=0).astype(np.float32)
```
